# Optimizing a Trainium2 kernel written in Bass

```python
import jax
import jax.numpy as jnp
from jax import lax
import numpy as np

D_MODEL = 1024
BATCH = 16
SEQ = 2048
DEPTH = 1

EPS = 1e-6
N_MOD = 6
RET_HEADS = 4
RET_QK_DIM = 256
RET_V_DIM = 512
RET_QK = RET_HEADS * RET_QK_DIM
RET_V = RET_HEADS * RET_V_DIM
RET_CHUNK = 128
ROPE_BASE = 10000.0
SSM_D_INNER = 2 * D_MODEL
SSM_HEAD_DIM = 64
SSM_HEADS = SSM_D_INNER // SSM_HEAD_DIM
SSM_GROUPS = 8
SSM_STATE = 128
SSM_CONV = 4
SSM_CONV_DIM = SSM_D_INNER + 2 * SSM_GROUPS * SSM_STATE
SSM_CHUNK = 128
N_EXPERTS = 32
TOP_K = 4
D_FF = D_MODEL
SWIGLU_LIMIT = 7.0
SWIGLU_ALPHA = 1.702
MOE_BLOCK = 128
IN_WIDTHS = (RET_QK, RET_QK, RET_V, RET_V, SSM_D_INNER, SSM_CONV_DIM, SSM_HEADS, D_MODEL, D_MODEL)
D_IN_PROJ = RET_QK * 2 + RET_V * 2 + SSM_D_INNER + SSM_CONV_DIM + SSM_HEADS + 2 * D_MODEL

kernel_name = 'hybrid_retention_ssd_moe_block'


def _split_points(widths):
    pts, acc = [], 0
    for w in widths[:-1]:
        acc += w
        pts.append(acc)
    return pts


def rmsnorm(x, w):
    xf = x.astype(jnp.float32)
    y = xf * lax.rsqrt(jnp.mean(xf * xf, axis=-1, keepdims=True) + EPS)
    return (y * w.astype(jnp.float32)).astype(x.dtype)


def rope(t, pos):
    half = t.shape[-1] // 2
    inv_freq = ROPE_BASE ** (-jnp.arange(half, dtype=jnp.float32) / half)
    ang = pos[:, None] * inv_freq[None, :]
    cos = jnp.cos(ang)[None, :, None, :]
    sin = jnp.sin(ang)[None, :, None, :]
    t = t.astype(jnp.float32)
    t1, t2 = t[..., :half], t[..., half:]
    return jnp.concatenate([t1 * cos - t2 * sin, t1 * sin + t2 * cos], axis=-1)


def to_chunks(t, chunk):
    b, s = t.shape[:2]
    return jnp.moveaxis(t.reshape((b, s // chunk, chunk) + t.shape[2:]), 1, 0)


def from_chunks(t):
    n, b, c = t.shape[:3]
    return jnp.moveaxis(t, 0, 1).reshape((b, n * c) + t.shape[3:])


def retention_chunkwise(q, k, v):
    b, s, h, dk = q.shape
    dv = v.shape[-1]
    c = RET_CHUNK
    log_gamma = jnp.log(1.0 - 2.0 ** (-5.0 - jnp.arange(h, dtype=jnp.float32)))
    idx = jnp.arange(c, dtype=jnp.float32)
    rel = idx[:, None] - idx[None, :]
    causal = rel >= 0
    decay_in = jnp.where(causal[None], jnp.exp(jnp.where(causal, rel, 0.0)[None] * log_gamma[:, None, None]), 0.0)
    decay_q = jnp.exp((idx + 1.0)[:, None] * log_gamma[None, :])
    decay_k = jnp.exp((c - 1.0 - idx)[:, None] * log_gamma[None, :])
    decay_c = jnp.exp(c * log_gamma)

    def step(state, inp):
        qc, kc, vc = inp
        scores = jnp.einsum('blhd,bshd->bhls', qc, kc) * decay_in[None]
        inner = jnp.einsum('bhls,bshv->blhv', scores, vc)
        cross = jnp.einsum('blhd,bhdv->blhv', qc, state) * decay_q[None, :, :, None]
        state = state * decay_c[None, :, None, None] + jnp.einsum(
            'bshd,bshv->bhdv', kc * decay_k[None, :, :, None], vc)
        return state, inner + cross

    state0 = jnp.zeros((b, h, dk, dv), jnp.float32)
    _, out = lax.scan(step, state0, (to_chunks(q, c), to_chunks(k, c), to_chunks(v, c)))
    return from_chunks(out)


def ssd_chunked(x, dt, a, bm, cm):
    b, s, h, p = x.shape
    g, n = bm.shape[2], bm.shape[3]
    j = h // g
    c = SSM_CHUNK
    xdt = to_chunks((x * dt[..., None]).reshape(b, s, g, j, p), c)
    adt = to_chunks((dt * a).reshape(b, s, g, j), c)
    causal = jnp.tril(jnp.ones((c, c), dtype=bool))[None, :, :, None, None]

    def step(state, inp):
        xc, ac, bc, cc = inp
        acs = jnp.cumsum(ac, axis=1)
        seg = jnp.exp(jnp.where(causal, acs[:, :, None] - acs[:, None, :], -jnp.inf))
        cb = jnp.einsum('blgn,bsgn->blsg', cc, bc)
        y_diag = jnp.einsum('blsgj,bsgjp->blgjp', cb[..., None] * seg, xc)
        y_off = jnp.einsum('blgn,bgjpn->blgjp', cc, state) * jnp.exp(acs)[..., None]
        decay_s = jnp.exp(acs[:, -1:] - acs)
        state = state * jnp.exp(acs[:, -1])[..., None, None] + jnp.einsum(
            'bsgn,bsgj,bsgjp->bgjpn', bc, decay_s, xc)
        return state, y_diag + y_off

    state0 = jnp.zeros((b, g, j, p, n), jnp.float32)
    _, y = lax.scan(step, state0, (xdt, adt, to_chunks(bm, c), to_chunks(cm, c)))
    return from_chunks(y).reshape(b, s, h, p)


def causal_depthwise_conv(u, w, bias):
    k = w.shape[0]
    out = lax.conv_general_dilated(
        u, w[:, None, :].astype(u.dtype), window_strides=(1,), padding=[(k - 1, 0)],
        dimension_numbers=('NWC', 'WIO', 'NWC'), feature_group_count=u.shape[-1])
    return out + bias


def gated_group_rmsnorm(y, z, w):
    b, s, d = y.shape
    yz = y.astype(jnp.float32) * jax.nn.silu(z.astype(jnp.float32))
    grp = yz.reshape(b, s, SSM_GROUPS, d // SSM_GROUPS)
    grp = grp * lax.rsqrt(jnp.mean(grp * grp, axis=-1, keepdims=True) + EPS)
    return (grp.reshape(b, s, d) * w.astype(jnp.float32)).astype(z.dtype)


def hybrid_mixer(h, w_in, conv_w, conv_b, dt_bias, a_log, d_skip, ssm_norm,
                 w_ret_out, w_ssm_out, w_out):
    b, s, _ = h.shape
    proj = h @ w_in
    q, k, v, g, z, xbc, dt, gate_a, gate_b = jnp.split(proj, _split_points(IN_WIDTHS), axis=-1)

    pos = jnp.arange(s, dtype=jnp.float32)
    q = rope(q.reshape(b, s, RET_HEADS, RET_QK_DIM), pos)
    k = rope(k.reshape(b, s, RET_HEADS, RET_QK_DIM), pos) * (RET_QK_DIM ** -0.5)
    v = v.reshape(b, s, RET_HEADS, RET_V_DIM).astype(jnp.float32)
    ret = retention_chunkwise(q, k, v)
    ret = ret * lax.rsqrt(jnp.mean(ret * ret, axis=-1, keepdims=True) + EPS)
    ret = ret.reshape(b, s, RET_V).astype(h.dtype) * jax.nn.silu(g)
    y_a = ret @ w_ret_out

    xbc = jax.nn.silu(causal_depthwise_conv(xbc, conv_w, conv_b))
    xs, bm, cm = jnp.split(xbc, [SSM_D_INNER, SSM_D_INNER + SSM_GROUPS * SSM_STATE], axis=-1)
    dt = jax.nn.softplus(dt.astype(jnp.float32) + dt_bias.astype(jnp.float32))
    a = -jnp.exp(a_log.astype(jnp.float32))
    xs = xs.reshape(b, s, SSM_HEADS, SSM_HEAD_DIM).astype(jnp.float32)
    bm = bm.reshape(b, s, SSM_GROUPS, SSM_STATE).astype(jnp.float32)
    cm = cm.reshape(b, s, SSM_GROUPS, SSM_STATE).astype(jnp.float32)
    y = ssd_chunked(xs, dt, a, bm, cm) + d_skip.astype(jnp.float32)[:, None] * xs
    y = gated_group_rmsnorm(y.reshape(b, s, SSM_D_INNER), z, ssm_norm)
    y_b = y @ w_ssm_out

    merged = jax.nn.sigmoid(gate_a) * y_a + jax.nn.sigmoid(gate_b) * y_b
    return merged @ w_out


def moe_ffn(h, w_router, b_router, w_gate_up, b_gate_up, w_down, b_down):
    b, s, d = h.shape
    t = b * s
    tk = t * TOP_K
    hf = h.reshape(t, d)
    logits = (hf @ w_router).astype(jnp.float32) + b_router.astype(jnp.float32)
    top_vals, top_idx = lax.top_k(logits, TOP_K)
    probs = jax.nn.softmax(top_vals, axis=-1)

    e_flat = top_idx.reshape(-1).astype(jnp.int32)
    w_flat = probs.reshape(-1)
    tok_flat = jnp.arange(tk, dtype=jnp.int32) // TOP_K
    order = jnp.argsort(e_flat)
    sorted_e, sorted_tok, sorted_w = e_flat[order], tok_flat[order], w_flat[order]
    counts = jnp.bincount(e_flat, length=N_EXPERTS).astype(jnp.int32)
    padded = ((counts + MOE_BLOCK - 1) // MOE_BLOCK) * MOE_BLOCK
    start_sorted = jnp.cumsum(counts) - counts
    pad_end = jnp.cumsum(padded)
    start_pad = pad_end - padded
    dest = start_pad[sorted_e] + jnp.arange(tk, dtype=jnp.int32) - start_sorted[sorted_e]
    n_blocks = (tk + MOE_BLOCK - 1) // MOE_BLOCK + N_EXPERTS
    n_rows = n_blocks * MOE_BLOCK
    tok_buf = jnp.full((n_rows,), t, jnp.int32).at[dest].set(sorted_tok)
    w_buf = jnp.zeros((n_rows,), jnp.float32).at[dest].set(sorted_w)
    block_start = jnp.arange(n_blocks, dtype=jnp.int32) * MOE_BLOCK
    block_expert = jnp.minimum(jnp.searchsorted(pad_end, block_start, side='right'),
                               N_EXPERTS - 1).astype(jnp.int32)
    x_pad = jnp.concatenate([hf, jnp.zeros((1, d), hf.dtype)], axis=0)

    def step(acc, inp):
        tok, wt, e = inp
        xb = x_pad[tok]
        gu = xb @ w_gate_up[e] + b_gate_up[e]
        gate = jnp.minimum(gu[:, :D_FF], SWIGLU_LIMIT)
        up = jnp.clip(gu[:, D_FF:], -SWIGLU_LIMIT, SWIGLU_LIMIT)
        act = gate * jax.nn.sigmoid(SWIGLU_ALPHA * gate) * (up + 1.0)
        yb = act @ w_down[e] + b_down[e]
        acc = acc.at[tok].add(yb.astype(jnp.float32) * wt[:, None])
        return acc, None

    acc0 = jnp.zeros((t + 1, d), jnp.float32)
    acc, _ = lax.scan(step, acc0, (tok_buf.reshape(n_blocks, MOE_BLOCK),
                                   w_buf.reshape(n_blocks, MOE_BLOCK), block_expert))
    return acc[:t].reshape(b, s, d).astype(h.dtype)


def setup_inputs(seed: int = 0) -> dict:
    key = jax.random.key(seed)
    ks = jax.random.split(key, 24)
    f32 = jnp.float32
    L = DEPTH

    def nrm(k, shape, scale):
        return jax.random.normal(k, shape, f32) * scale

    dt0 = jnp.exp(jax.random.uniform(ks[9], (L, SSM_HEADS), f32) * (jnp.log(0.1) - jnp.log(0.001)) + jnp.log(0.001))
    return {
        'x': nrm(ks[0], (BATCH, SEQ, D_MODEL), 1.0),
        'c': nrm(ks[1], (BATCH, D_MODEL), 1.0),
        'w_ada': nrm(ks[2], (L, D_MODEL, N_MOD * D_MODEL), 0.5 * D_MODEL ** -0.5),
        'b_ada': nrm(ks[3], (L, N_MOD * D_MODEL), 0.02),
        'norm_mix': 1.0 + nrm(ks[4], (L, D_MODEL), 0.02),
        'norm_ffn': 1.0 + nrm(ks[5], (L, D_MODEL), 0.02),
        'w_in': nrm(ks[6], (L, D_MODEL, D_IN_PROJ), D_MODEL ** -0.5),
        'conv_w': nrm(ks[7], (L, SSM_CONV, SSM_CONV_DIM), SSM_CONV ** -0.5),
        'conv_b': nrm(ks[8], (L, SSM_CONV_DIM), 0.02),
        'dt_bias': dt0 + jnp.log(-jnp.expm1(-dt0)),
        'a_log': jnp.log(jax.random.uniform(ks[10], (L, SSM_HEADS), f32, 1.0, 16.0)),
        'd_skip': 1.0 + nrm(ks[11], (L, SSM_HEADS), 0.02),
        'ssm_norm': 1.0 + nrm(ks[12], (L, SSM_D_INNER), 0.02),
        'w_ret_out': nrm(ks[13], (L, RET_V, D_MODEL), RET_V ** -0.5),
        'w_ssm_out': nrm(ks[14], (L, SSM_D_INNER, D_MODEL), SSM_D_INNER ** -0.5),
        'w_out': nrm(ks[15], (L, D_MODEL, D_MODEL), D_MODEL ** -0.5),
        'w_router': nrm(ks[16], (L, D_MODEL, N_EXPERTS), D_MODEL ** -0.5),
        'b_router': nrm(ks[17], (L, N_EXPERTS), 0.01),
        'w_gate_up': nrm(ks[18], (L, N_EXPERTS, D_MODEL, 2 * D_FF), D_MODEL ** -0.5),
        'b_gate_up': nrm(ks[19], (L, N_EXPERTS, 2 * D_FF), 0.02),
        'w_down': nrm(ks[20], (L, N_EXPERTS, D_FF, D_MODEL), D_FF ** -0.5),
        'b_down': nrm(ks[21], (L, N_EXPERTS, D_MODEL), 0.02),
        'norm_final': 1.0 + nrm(ks[22], (D_MODEL,), 0.02),
    }


def reference(x, c, w_ada, b_ada, norm_mix, norm_ffn, w_in, conv_w, conv_b, dt_bias,
              a_log, d_skip, ssm_norm, w_ret_out, w_ssm_out, w_out, w_router, b_router,
              w_gate_up, b_gate_up, w_down, b_down, norm_final):
    cond = jax.nn.silu(c)
    for l in range(DEPTH):
        mod = (cond @ w_ada[l] + b_ada[l])[:, None, :]
        sh_m, sc_m, g_m, sh_f, sc_f, g_f = jnp.split(mod, N_MOD, axis=-1)
        h = rmsnorm(x, norm_mix[l]) * (1.0 + sc_m) + sh_m
        x = x + g_m * hybrid_mixer(h, w_in[l], conv_w[l], conv_b[l], dt_bias[l], a_log[l],
                                   d_skip[l], ssm_norm[l], w_ret_out[l], w_ssm_out[l], w_out[l])
        h = rmsnorm(x, norm_ffn[l]) * (1.0 + sc_f) + sh_f
        x = x + g_f * moe_ffn(h, w_router[l], b_router[l], w_gate_up[l], b_gate_up[l],
                              w_down[l], b_down[l])
    return rmsnorm(x, norm_final)
```

```python
import os
import numpy as np
import concourse.bass as bass
import concourse.mybir as mybir
from concourse.bass_utils import run_bass_kernel_spmd
from contextlib import ExitStack

F32 = mybir.dt.float32
BF16 = mybir.dt.bfloat16
AF = mybir.ActivationFunctionType
ALU = mybir.AluOpType
AX = mybir.AxisListType

NSLOT = 6
D = 1024
DIN = 14368
NEXP = 32
EPS = 1e-6


class Tl:
    def __init__(self, h, name=""):
        self.h = h
        self.name = name
        self.w = None
        self.r = []

    def __getitem__(self, k):
        return self.h[k]


class V:
    def __init__(self, tl, ap):
        self.tl = tl
        self.ap = ap

    def __getitem__(self, k):
        return self.ap[k]


def _norm(ts):
    return [getattr(t, "tl", t) for t in ts]


class Sched:
    ENG = ("pe", "act", "dve", "pool", "sp")

    def __init__(self, nc, es):
        self.nc = nc
        self.es = es
        self.engobj = {"pe": nc.tensor, "act": nc.scalar, "dve": nc.vector, "pool": nc.gpsimd, "sp": nc.sync}
        self.sems = {}
        self.cnt = {}
        for e in ("pe", "act", "dve", "pool"):
            self.sems[e] = es.enter_context(nc.semaphore("s_" + e))
            self.cnt[e] = 0
        self.dq = {}
        for q in ("sp", "pool"):
            self.dq[q] = 0
            for s in range(NSLOT):
                key = "d_%s_%d" % (q, s)
                self.sems[key] = es.enter_context(nc.semaphore(key))
                self.cnt[key] = 0
        self.waited = {e: {} for e in self.ENG}
        self.ninstr = {e: 0 for e in self.ENG}

    def sb(self, name, shape, dt, es=None):
        es = es or self.es
        self.uid = getattr(self, "uid", 0) + 1
        name = "sb%d_%s" % (self.uid, name)
        return Tl(es.enter_context(self.nc.sbuf_tensor(name, list(shape), dt)), name)

    def ps(self, name, shape, dt=F32):
        return Tl(self.es.enter_context(self.nc.psum_tensor(name, list(shape), dt)), name)

    def _deps(self, reads, writes):
        deps = []
        for t in reads:
            if t.w is not None:
                deps.append(t.w)
        for t in writes:
            if t.w is not None:
                deps.append(t.w)
            deps.extend(t.r)
        return deps

    def _waits(self, eng, deps):
        need = {}
        for (key, c) in deps:
            if eng == "pe" and key == "pe":
                continue
            if self.waited[eng].get(key, 0) < c and need.get(key, 0) < c:
                need[key] = c
        out = []
        for key, c in need.items():
            self.waited[eng][key] = c
            out.append((self.sems[key], c))
        return out

    def _issue(self, engname, waits, fn, sem, inc):
        eng = self.engobj[engname]
        for (s, c) in waits:
            eng.wait_ge(s, c)
        if fn is not None:
            fn(eng).then_inc(sem, inc)
        self.ninstr[engname] += 1

    def op(self, eng, fn, reads=(), writes=()):
        reads, writes = _norm(reads), _norm(writes)
        waits = self._waits(eng, self._deps(reads, writes))
        self.cnt[eng] += 1
        c = self.cnt[eng]
        self._issue(eng, waits, fn, self.sems[eng], 1)
        for t in reads:
            t.r.append((eng, c))
            if len(t.r) > 64:
                t.r = t.r[-48:] if False else self._compact(t.r)
        for t in writes:
            t.w = (eng, c)
            t.r = []
        return c

    @staticmethod
    def _compact(r):
        best = {}
        for (k, c) in r:
            if best.get(k, 0) < c:
                best[k] = c
        return list(best.items())

    def dma(self, q, fn, reads=(), writes=()):
        reads, writes = _norm(reads), _norm(writes)
        slot = self.dq[q] % NSLOT
        self.dq[q] += 1
        key = "d_%s_%d" % (q, slot)
        deps = self._deps(reads, writes)
        if self.cnt[key] > 0:
            deps.append((key, self.cnt[key]))
        waits = self._waits(q, deps)
        self.cnt[key] += 16
        c = self.cnt[key]
        self._issue(q, waits, fn, self.sems[key], 16)
        for t in reads:
            t.r.append((key, c))
        for t in writes:
            t.w = (key, c)
            t.r = []
        return c

    def barrier(self):
        for e in self.ENG:
            deps = [(k, c) for k, c in self.cnt.items() if c > 0]
            waits = self._waits(e, deps)
            if waits:
                self._issue(e, waits, None, None, 0)

    def finish(self):
        deps = [(k, c) for k, c in self.cnt.items() if c > 0]
        self._issue("sp", self._waits("sp", deps), None, None, 0)


def host_consts(S):
    nt = S // 256
    half = 128
    inv_freq = (10000.0 ** (-np.arange(half, dtype=np.float32) / half)).astype(np.float32)
    pos = np.arange(S, dtype=np.float32)
    ang = (pos[None, :] * inv_freq[:, None]).astype(np.float32)
    cos = np.cos(ang).astype(np.float32)
    sin = np.sin(ang).astype(np.float32)
    rope = np.zeros((nt, 128, 1024), np.float32)
    for t in range(nt):
        c = cos[:, t * 256:(t + 1) * 256]
        s = sin[:, t * 256:(t + 1) * 256]
        rope[t] = np.concatenate([c, s, s, c], axis=1)
    gam = 1.0 - 2.0 ** (-5.0 - np.arange(4, dtype=np.float64))
    idx = np.arange(128, dtype=np.float64)
    decinT = np.zeros((128, 512), np.float64)
    decq = np.zeros((128, 512), np.float64)
    deck = np.zeros((128, 4), np.float64)
    for h in range(4):
        rel = idx[None, :] - idx[:, None]
        decinT[:, h * 128:(h + 1) * 128] = np.where(rel >= 0, gam[h] ** np.maximum(rel, 0), 0.0) / 16.0
        decq[:, h * 128:(h + 1) * 128] = (gam[h] ** (idx + 1.0))[None, :]
        deck[:, h] = gam[h] ** (127.0 - idx) / 16.0
    gC = [float(gam[h] ** 128.0) for h in range(4)]
    tri = (idx[:, None] <= idx[None, :]).astype(np.float32)
    nm1 = np.where(idx[None, :] < idx[:, None], -30000.0, 0.0)
    nm = np.tile(nm1, (1, 4)).astype(np.float32)
    return dict(rope=rope, decinT=decinT.astype(np.float32), decq=decq.astype(np.float32),
                deck=deck.astype(np.float32), tri=tri, nm=nm,
                identf=np.eye(128, dtype=np.float32)), gC


def build(NSEQ, S, gC, dbg=False):
    nc = bass.Bass("TRN2", target_bir_lowering=False)
    U = min(1024, S)
    NU = S // U
    TPU = U // 256
    NTB = U // 128
    NTG = U // 512 if U >= 512 else 1
    TGW = min(512, U)

    def din(name, shape):
        return nc.dram_tensor(name, list(shape), F32, kind="ExternalInput").ap()

    x_d = din("x", [NSEQ, S, D])
    cl_d = din("cl", [NSEQ, 128, 8])
    wada_d = din("w_ada", [D, 6 * D])
    bada_d = din("b_ada", [1, 6 * D])
    nmix_d = din("norm_mix", [1, D])
    nffn_d = din("norm_ffn", [1, D])
    nfin_d = din("norm_final", [1, D])
    ssmn_d = din("ssm_norm", [1, 2048])
    dtb_d = din("dt_bias", [1, 32])
    alog_d = din("a_log", [1, 32])
    dsk_d = din("d_skip", [1, 32])
    win_d = din("w_in", [D, DIN])
    cw_d = din("cw", [128, 32, 4])
    cb_d = din("cb", [128, 32])
    wro_d = din("w_ret_out", [2048, D])
    wso_d = din("w_ssm_out", [2048, D])
    wo_d = din("w_out", [D, D])
    wr_d = din("w_router", [D, NEXP])
    br_d = din("b_router", [1, NEXP])
    wgu_d = din("w_gate_up", [NEXP, D, 2 * D])
    bgu_d = din("bgu", [128, NEXP, 16])
    wd_d = din("w_down", [NEXP, D, D])
    bd_d = din("b_down", [NEXP, D])
    rope_d = din("rope", [S // 256, 128, 1024])
    decinT_d = din("decinT", [128, 512])
    decq_d = din("decq", [128, 512])
    deck_d = din("deck", [128, 4])
    tri_d = din("tri", [128, 128])
    nm_d = din("nm", [128, 512])
    identf_d = din("identf", [128, 128])
    out_d = nc.dram_tensor("out", [NSEQ, S, D], F32, kind="ExternalOutput").ap()
    dbg_d = {}
    if dbg:
        for nm_, shp in (("d_x1", [NSEQ, S, D]), ("d_retT", [128, 16, 256]), ("d_yT", [128, 16, 256]),
                         ("d_gw", [128, NTB, 32]), ("d_hT", [128, 8, 256]), ("d_qT", [128, 8, 256]),
                         ("d_kT", [128, 8, 256]), ("d_v", [128, 2, 2048]), ("d_mod", [128, 3, 1024]),
                         ("d_xs", [128, 2, 2048]), ("d_dt", [128, 2, 32]), ("d_bmT", [128, 8, 256]),
                         ("d_cmT", [128, 8, 256])):
            dbg_d[nm_] = nc.dram_tensor(nm_, shp, F32, kind="ExternalOutput").ap()

    with ExitStack() as es:
        k = Sched(nc, es)
        PFl = [k.ps("pf%d" % i, [128, 512], F32) for i in range(6)]
        PBl = [k.ps("pb%d" % i, [128, 1024], BF16) for i in range(2)]
        st = {"pf": 0, "pb": 0, "w": 0, "dbgc": 0}

        def pf():
            t = PFl[st["pf"] % 6]
            st["pf"] += 1
            assert t.w is None or len(t.r) > 0, "psum bank reused before consumption"
            return t

        def pb():
            t = PBl[st["pb"] % 2]
            st["pb"] += 1
            assert t.w is None or len(t.r) > 0
            return t

        def bc(ap, n=128):
            return ap.partition_broadcast(n)

        def dbg_out(name, src_ap_fn, tl, shape, is_bf=False):
            if not dbg or name not in dbg_d:
                return
            if "stg" not in st:
                st["stg"] = k.sb("dbgstg", [128, 4096], F32, st["esm"])
            stg = st["stg"]
            n = int(np.prod(shape[1:]))
            if len(shape) == 3:
                sv = stg[:, 0:n].rearrange("p (a b) -> p a b", a=shape[1])
            else:
                sv = stg[:, 0:n]
            k.op("dve", lambda e: e.tensor_copy(out=sv, in_=src_ap_fn()), reads=[tl], writes=[stg])
            k.dma("sp", lambda e: e.dma_start(out=dbg_d[name], in_=sv), reads=[stg])

        ident_b = k.sb("ident_b", [128, 128], BF16)
        ident_f = k.sb("ident_f", [128, 128], F32)
        ones_f = k.sb("ones_f", [128, 128], F32)
        ones_b = k.sb("ones_b", [1, 128], BF16)
        tri_f = k.sb("tri_f", [128, 128], F32)
        nm_f = k.sb("nm_f", [128, 512], F32)
        decinT = k.sb("decinT", [128, 512], F32)
        decq = k.sb("decq", [128, 512], F32)
        deck = k.sb("deck", [128, 4], F32)
        cw = k.sb("cw", [128, 32, 4], F32)
        cb = k.sb("cb", [128, 32], F32)
        a_bc = k.sb("a_bc", [128, 32], F32)
        dtb_bc = k.sb("dtb_bc", [128, 32], F32)
        dsk_bc = k.sb("dsk_bc", [128, 32], F32)
        ssmn_bc = k.sb("ssmn_bc", [128, 2048], BF16)
        epsb = k.sb("epsb", [128, 1], F32)
        oneb = k.sb("oneb", [128, 1], F32)

        k.dma("pool", lambda e: e.dma_start(out=ident_b[:], in_=identf_d), writes=[ident_b])
        k.dma("pool", lambda e: e.dma_start(out=ssmn_bc[:], in_=bc(ssmn_d)), writes=[ssmn_bc])
        for (t_, d_) in ((ident_f, identf_d), (tri_f, tri_d), (nm_f, nm_d), (decinT, decinT_d), (decq, decq_d),
                         (deck, deck_d), (cw, cw_d), (cb, cb_d)):
            k.dma("sp", (lambda t_, d_: (lambda e: e.dma_start(out=t_[:], in_=d_)))(t_, d_), writes=[t_])
        for (t_, d_) in ((a_bc, alog_d), (dtb_bc, dtb_d), (dsk_bc, dsk_d)):
            k.dma("sp", (lambda t_, d_: (lambda e: e.dma_start(out=t_[:], in_=bc(d_))))(t_, d_), writes=[t_])
        k.op("dve", lambda e: e.memset(ones_f[:], 1.0), writes=[ones_f])
        k.op("dve", lambda e: e.memset(ones_b[:], 1.0), writes=[ones_b])
        k.op("dve", lambda e: e.memset(epsb[:], EPS), writes=[epsb])
        k.op("dve", lambda e: e.memset(oneb[:], 1.0), writes=[oneb])
        k.op("act", lambda e: e.activation(out=a_bc[:], in_=a_bc[:], func=AF.Exp), reads=[a_bc], writes=[a_bc])
        k.op("dve", lambda e: e.tensor_scalar(out=a_bc[:], in0=a_bc[:], scalar1=-1.0, scalar2=None, op0=ALU.mult),
             reads=[a_bc], writes=[a_bc])

        wscr_d = nc.dram_tensor("wscr", [38, 128, 4096], BF16, kind="Internal").ap()
        scrT = Tl(None, "wscr")
        blocks = []
        for n in range(24):
            blocks.append(win_d[:, n * 512:(n + 1) * 512])
        for j in range(4):
            blocks.append(win_d[:, 12320 + j * 512:12320 + (j + 1) * 512])
        for wsrc in (wro_d, wso_d):
            for nb in range(2):
                for kh in range(2):
                    blocks.append(wsrc[kh * 1024:(kh + 1) * 1024, nb * 512:(nb + 1) * 512])
        for nb in range(2):
            blocks.append(wo_d[:, nb * 512:(nb + 1) * 512])
        with ExitStack() as esp:
            pstg = [k.sb("pstg%d" % i, [128, 8, 512], F32, esp) for i in range(3)]
            pcb = [k.sb("pcb%d" % i, [128, 8, 512], BF16, esp) for i in range(3)]
            for i, src in enumerate(blocks):
                sg_, cb_ = pstg[i % 3], pcb[i % 3]
                k.dma("sp", lambda e: e.dma_start(out=sg_[:], in_=src.rearrange("(kc p) c -> p kc c", p=128)), writes=[sg_])
                if i % 2 == 0:
                    k.op("act", lambda e: e.copy(out=cb_[:], in_=sg_[:]), reads=[sg_], writes=[cb_])
                else:
                    k.op("dve", lambda e: e.tensor_copy(out=cb_[:], in_=sg_[:]), reads=[sg_], writes=[cb_])
                k.dma("sp", lambda e: e.dma_start(out=wscr_d[i], in_=cb_[:].rearrange("p a b -> p (a b)")), reads=[cb_], writes=[scrT])
            k.barrier()
        if os.environ.get("MK_STOP") == "precast":
            k.finish()
            return nc

        srf_d = nc.dram_tensor("st_rf", [128, 8 * 512], F32, kind="Internal").ap()
        ssf_d = nc.dram_tensor("st_sf", [128, 2048], F32, kind="Internal").ap()
        shl_d = nc.dram_tensor("st_hl", [128, 96], F32, kind="Internal").ap()
        acc = k.sb("acc", [128, NTB, D], F32)
        modm = [k.sb("modm%d" % i, [128, D], F32) for i in range(3)]
        cs = k.sb("cs", [128, 8], F32)
        brow = k.sb("brow", [1, 256], F32)
        ssq = k.sb("ssq", [128, 8], F32)
        rstd = k.sb("rstd", [128, 8], F32)
        sqj = k.sb("sqj", [128, D], BF16)
        hold = {"htmp": None}
        hbf = k.sb("hbf", [128, D], BF16)

        def rms_rstd(src_ap, src_tl, ncols, col):
            k.op("dve", lambda e: e.memset(ssq[:, col:col + 1], 0.0), writes=[ssq])
            k.op("act", lambda e: e.activation(out=sqj[:, 0:ncols], in_=src_ap, func=AF.Square,
                                               accum_out=ssq[:, col:col + 1]),
                 reads=[src_tl, ssq], writes=[sqj, ssq])
            k.op("act", lambda e: e.activation(out=rstd[:, col:col + 1], in_=ssq[:, col:col + 1], func=AF.Ln,
                                               scale=1.0 / ncols, bias=epsb[:]), reads=[ssq, epsb], writes=[rstd])
            k.op("act", lambda e: e.activation(out=rstd[:, col:col + 1], in_=rstd[:, col:col + 1], func=AF.Exp,
                                               scale=-0.5), reads=[rstd], writes=[rstd])

        def norm_mod_T(tb, gamma, shift, dstT, dcol):
            rms_rstd(acc[:, tb, :], acc, D, 0)
            htmp = hold["htmp"]
            k.op("dve", lambda e: e.scalar_tensor_tensor(out=htmp[:], in0=acc[:, tb, :], scalar=rstd[:, 0:1],
                                                         in1=gamma[:], op0=ALU.mult, op1=ALU.mult),
                 reads=[acc, rstd, gamma], writes=[htmp])
            k.op("dve", lambda e: e.tensor_tensor(out=hbf[:], in0=htmp[:], in1=shift[:], op=ALU.add),
                 reads=[htmp, shift], writes=[hbf])
            P = pb()

            def fn(e):
                for j in range(8):
                    ins = e.transpose(P[:, j * 128:(j + 1) * 128], hbf[:, j * 128:(j + 1) * 128], ident_b[:])
                return ins
            k.op("pe", fn, reads=[hbf, ident_b], writes=[P])
            k.op("act", lambda e: e.copy(out=dstT[:, :, dcol:dcol + 128],
                                         in_=P[:].rearrange("p (j t) -> p j t", j=8)), reads=[P], writes=[dstT])

        def compute_mod(b, j0, dst, condbc, wst):
            for i in range(3):
                for q4 in range(4):
                    c0 = (j0 + i) * D + q4 * 256
                    k.dma("sp", lambda e: e.dma_start(out=wst[:], in_=wada_d[:, c0:c0 + 256].rearrange(
                        "(kc p) c -> p kc c", p=128)), writes=[wst])
                    k.dma("sp", lambda e: e.dma_start(out=brow[:], in_=bada_d[0:1, c0:c0 + 256]), writes=[brow])
                    P = pf()

                    def fn(e):
                        for kc in range(8):
                            e.matmul(P[:, 0:256], lhsT=condbc[:, kc, :], rhs=wst[:, kc, :], start=(kc == 0), stop=False)
                        return e.matmul(P[:, 0:256], lhsT=ones_f[0:1, :], rhs=brow[0:1, :], start=False, stop=True)
                    k.op("pe", fn, reads=[condbc, wst, ones_f, brow], writes=[P])
                    k.op("act", lambda e: e.copy(out=dst[i][:, q4 * 256:(q4 + 1) * 256], in_=P[:, 0:256]),
                         reads=[P], writes=[dst[i]])

        def wload(es_unused, wb, blk, ncols=512):
            t = wb[st["w"] % len(wb)]
            st["w"] += 1
            k.dma("sp", lambda e: e.dma_start(out=t[:].rearrange("p a b -> p (a b)"), in_=wscr_d[blk]), reads=[scrT], writes=[t])
            return t

        for b in range(NSEQ):
            k.dma("sp", lambda e: e.dma_start(out=cs[:], in_=cl_d[b]), writes=[cs])
            k.op("act", lambda e: e.activation(out=cs[:], in_=cs[:], func=AF.Silu), reads=[cs], writes=[cs])
            with ExitStack() as esi:
                condbc = k.sb("condbc", [128, 8, 128], F32, esi)
                wst = k.sb("wst", [128, 8, 256], F32, esi)
                nrm_bc = k.sb("nrm_bc", [128, D], F32, esi)
                k.op("dve", lambda e: e.tensor_copy(out=condbc[:], in_=cs[:].unsqueeze(2).broadcast_to([128, 8, 128])),
                     reads=[cs], writes=[condbc])
                compute_mod(b, 0, modm, condbc, wst)
                k.dma("sp", lambda e: e.dma_start(out=nrm_bc[:], in_=bc(nmix_d)), writes=[nrm_bc])
                k.op("dve", lambda e: e.scalar_tensor_tensor(out=modm[1][:], in0=modm[1][:], scalar=1.0, in1=nrm_bc[:],
                                                             op0=ALU.add, op1=ALU.mult), reads=[modm[1], nrm_bc], writes=[modm[1]])
                k.barrier()
            if dbg and b == 0:
                for i in range(3):
                    k.dma("sp", (lambda i: (lambda e: e.dma_start(out=dbg_d["d_mod"][:, i, :], in_=modm[i][:])))(i),
                          reads=[modm[i]])

            for u in range(NU):
                with ExitStack() as esm:
                    st["esm"] = esm
                    st.pop("stg", None)
                    hT = k.sb("hT", [128, 8, 256], BF16, esm)
                    wb = [k.sb("wb%d" % i, [128, 8, 512], BF16, esm) for i in range(3)]
                    R1 = k.sb("R1", [128, 12288], BF16, esm)
                    R2 = k.sb("R2", [128, 4096], BF16, esm)

                    def vw(T, a, b_, n):
                        return V(T, T[:, a:b_].rearrange("p (a b) -> p a b", a=n))
                    qT = vw(R1, 0, 2048, 8)
                    qsT = vw(R1, 2048, 4096, 8)
                    kT = vw(R1, 4096, 6144, 8)
                    ktm = vw(R1, 6144, 8192, 2)
                    vtm = vw(R1, 8192, 12288, 2)
                    xs = vw(R1, 0, 4096, 2)
                    bmT = vw(R1, 4096, 6144, 8)
                    bmtm = vw(R1, 6144, 8192, 2)
                    cmT = vw(R1, 8192, 10240, 8)
                    gaT = vw(R1, 0, 2048, 8)
                    gbT = vw(R1, 2048, 4096, 8)
                    mgT = vw(R1, 4096, 6144, 8)
                    retT = vw(R2, 0, 4096, 16)
                    yT = vw(R2, 0, 4096, 16)
                    sg = k.sb("sg", [128, 2, 2048], BF16, esm)
                    maT = k.sb("maT", [128, 8, 256], BF16, esm)
                    rst_f = k.sb("rst_f", [128, 8, 512], F32, esm)
                    rst_b = k.sb("rst_b", [128, 8, 512], BF16, esm)
                    sst_f = [k.sb("sst_f%d" % i, [128, 256], F32, esm) for i in range(8)]
                    sst_b = [k.sb("sst_b%d" % i, [128, 256], BF16, esm) for i in range(8)]
                    halo = k.sb("halo", [128, 32, 3], F32, esm)
                    if u == 0:
                        for t_ in [rst_f, rst_b, halo] + sst_f + sst_b:
                            k.op("dve", (lambda t_: (lambda e: e.memset(t_[:], 0.0)))(t_), writes=[t_])
                    else:
                        k.dma("sp", lambda e: e.dma_start(out=rst_f[:].rearrange("p a b -> p (a b)"), in_=srf_d), writes=[rst_f])
                        for i in range(8):
                            k.dma("sp", lambda e: e.dma_start(out=sst_f[i][:], in_=ssf_d[:, i * 256:(i + 1) * 256]), writes=[sst_f[i]])
                        k.dma("sp", lambda e: e.dma_start(out=halo[:].rearrange("p a b -> p (a b)"), in_=shl_d), writes=[halo])
                        k.op("act", lambda e: e.copy(out=rst_b[:], in_=rst_f[:]), reads=[rst_f], writes=[rst_b])
                        for i in range(8):
                            k.op("act", lambda e: e.copy(out=sst_b[i][:], in_=sst_f[i][:]), reads=[sst_f[i]], writes=[sst_b[i]])
                    ropet = k.sb("ropet", [128, 1024], F32, esm)
                    print("sbuf remaining (mixer, before temps)", nc.sbuf_bytes_remaining, flush=True)
                    rAB = k.sb("rAB", [128, 1024], F32, esm)
                    rA = V(rAB, rAB[:, 0:512])
                    rB = V(rAB, rAB[:, 512:1024])
                    hold["htmp"] = V(rAB, rAB[:, 0:1024])
                    sm = k.sb("sm", [128, 512], BF16, esm)
                    rg = k.sb("rg", [128, 512], BF16, esm)
                    craw = [k.sb("craw%d" % i, [128, 259], F32, esm) for i in range(2)]
                    cacc = [k.sb("cacc%d" % i, [128, 256], F32, esm) for i in range(2)]
                    cso = [k.sb("cso%d" % i, [128, 256], BF16, esm) for i in range(2)]
                    dtt = k.sb("dtt", [128, 2, 32], F32, esm)
                    adt = k.sb("adt", [128, 32], F32, esm)
                    acs = k.sb("acs", [128, 32], F32, esm)
                    nacs = k.sb("nacs", [128, 32], F32, esm)
                    eacs = k.sb("eacs", [128, 32], F32, esm)
                    etot = k.sb("etot", [128, 32], F32, esm)
                    dS = k.sb("dS", [128, 32], F32, esm)
                    cbT = k.sb("cbT", [128, 8, 128], BF16, esm)
                    Xg = [k.sb("Xg%d" % i, [128, 4, 128], F32, esm) for i in range(2)]
                    seg = [k.sb("seg%d" % i, [128, 4, 128], BF16, esm) for i in range(2)]
                    ssq2 = [k.sb("ssq2%d" % i, [128, 1], F32, esm) for i in range(2)]
                    rstd2 = [k.sb("rstd2%d" % i, [128, 1], F32, esm) for i in range(2)]
                    sqj2 = [k.sb("sqj2%d" % i, [128, 256], BF16, esm) for i in range(2)]
                    Mg = [k.sb("Mg%d" % i, [128, 4, 128], BF16, esm) for i in range(2)]
                    xdt = [k.sb("xdt%d" % i, [128, 256], BF16, esm) for i in range(2)]
                    xdS = [k.sb("xdS%d" % i, [128, 256], BF16, esm) for i in range(2)]
                    yt1_ = [k.sb("yt1%d" % i, [128, 256], F32, esm) for i in range(2)]
                    yt2_ = [k.sb("yt2%d" % i, [128, 256], F32, esm) for i in range(2)]
                    ynb_ = [k.sb("ynb%d" % i, [128, 256], BF16, esm) for i in range(2)]
                    wdt = k.sb("wdt", [128, 8, 32], BF16, esm)
                    otmp = rA
                    print("sbuf remaining (mixer)", nc.sbuf_bytes_remaining, flush=True)

                    k.dma("pool", lambda e: e.dma_start(out=wdt[:], in_=win_d[:, 12288:12320].rearrange(
                        "(kc p) c -> p kc c", p=128)), writes=[wdt])

                    for tt in range(TPU):
                        gt = u * TPU + tt
                        tok0 = gt * 256
                        tb0 = tt * 2
                        k.dma("sp", lambda e: e.dma_start(out=acc[:, tb0:tb0 + 2, :], in_=x_d[b, tok0:tok0 + 256, :].rearrange(
                            "(tc p) d -> p tc d", p=128)), writes=[acc])
                        k.dma("sp", lambda e: e.dma_start(out=ropet[:], in_=rope_d[gt]), writes=[ropet])
                        for tc in range(2):
                            norm_mod_T(tb0 + tc, modm[1], modm[0], hT, tc * 128)
                        if dbg and b == 0 and gt == 0:
                            dbg_out("d_hT", lambda: hT[:], hT, [128, 8, 256])

                        def fm_group(W, cc, P, col0):
                            def fn(e):
                                for kc in range(8):
                                    ins = e.matmul(P[:, col0:col0 + 256], lhsT=W[:, kc, cc * 128:(cc + 1) * 128],
                                                   rhs=hT[:, kc, :], start=(kc == 0), stop=(kc == 7))
                                return ins
                            k.op("pe", fn, reads=[W, hT], writes=[P])

                        def tm_group(W, tc, P, ncols):
                            def fn(e):
                                for kc in range(8):
                                    ins = e.matmul(P[:, 0:ncols], lhsT=hT[:, kc, tc * 128:(tc + 1) * 128],
                                                   rhs=W[:, kc, 0:ncols], start=(kc == 0), stop=(kc == 7))
                                return ins
                            k.op("pe", fn, reads=[W, hT], writes=[P])

                        for (blk0, dstT) in ((0, qT), (2, kT)):
                            for bi in range(2):
                                W = wload(esm, wb, blk0 + bi)
                                for hp in range(2):
                                    h = bi * 2 + hp
                                    P = pf()
                                    fm_group(W, hp * 2, P, 0)
                                    fm_group(W, hp * 2 + 1, P, 256)
                                    k.op("dve", lambda e: e.tensor_tensor(out=rA[:], in0=P[:], in1=ropet[:, 0:512], op=ALU.mult),
                                         reads=[P, ropet], writes=[rA])
                                    k.op("dve", lambda e: e.tensor_tensor(out=rB[:], in0=P[:], in1=ropet[:, 512:1024], op=ALU.mult),
                                         reads=[P, ropet], writes=[rB])
                                    k.op("dve", lambda e: e.tensor_tensor(out=dstT[:, 2 * h, :], in0=rA[:, 0:256], in1=rA[:, 256:512],
                                                                          op=ALU.subtract), reads=[rA], writes=[dstT])
                                    k.op("dve", lambda e: e.tensor_tensor(out=dstT[:, 2 * h + 1, :], in0=rB[:, 0:256], in1=rB[:, 256:512],
                                                                          op=ALU.add), reads=[rB], writes=[dstT])
                        for h in range(4):
                            for dc in range(2):
                                k.op("dve", lambda e: e.tensor_tensor(
                                    out=qsT[:, 2 * h + dc, :].rearrange("p (a l) -> p a l", a=2),
                                    in0=qT[:, 2 * h + dc, :].rearrange("p (a l) -> p a l", a=2),
                                    in1=decq[:, h * 128:(h + 1) * 128].unsqueeze(1).broadcast_to([128, 2, 128]),
                                    op=ALU.mult), reads=[qT, decq], writes=[qsT])
                        for tc in range(2):
                            P = pb()

                            def fn(e):
                                for j in range(8):
                                    ins = e.transpose(P[:, j * 128:(j + 1) * 128], kT[:, j, tc * 128:(tc + 1) * 128], ident_b[:])
                                return ins
                            k.op("pe", fn, reads=[kT, ident_b], writes=[P])
                            for h in range(4):
                                k.op("act", lambda e: e.activation(out=ktm[:, tc, h * 256:(h + 1) * 256], in_=P[:, h * 256:(h + 1) * 256],
                                                                   func=AF.Copy, scale=deck[:, h:h + 1]), reads=[P, deck], writes=[ktm])
                        if dbg and b == 0 and gt == 0:
                            dbg_out("d_qT", lambda: qT[:], qT, [128, 8, 256])
                            dbg_out("d_kT", lambda: kT[:], kT, [128, 8, 256])
                        for bi in range(4):
                            W = wload(esm, wb, 4 + bi)
                            for tc in range(2):
                                P = pf()
                                tm_group(W, tc, P, 512)
                                k.op("act", lambda e: e.copy(out=vtm[:, tc, bi * 512:(bi + 1) * 512], in_=P[:]), reads=[P], writes=[vtm])
                        for bi in range(4):
                            W = wload(esm, wb, 8 + bi)
                            for tc in range(2):
                                P = pf()
                                tm_group(W, tc, P, 512)
                                k.op("act", lambda e: e.activation(out=sg[:, tc, bi * 512:(bi + 1) * 512], in_=P[:], func=AF.Silu),
                                     reads=[P], writes=[sg])
                        if dbg and b == 0 and gt == 0:
                            dbg_out("d_v", lambda: vtm[:], vtm, [128, 2, 2048])
                        for tc in range(2):
                            tsl = slice(tc * 128, (tc + 1) * 128)
                            Ps = pf()

                            def fn(e):
                                for h in range(4):
                                    for dc in range(2):
                                        ins = e.matmul(Ps[:, h * 128:(h + 1) * 128], lhsT=kT[:, 2 * h + dc, tsl], rhs=qT[:, 2 * h + dc, tsl],
                                                       start=(dc == 0), stop=(dc == 1))
                                return ins
                            k.op("pe", fn, reads=[kT, qT], writes=[Ps])
                            k.op("dve", lambda e: e.tensor_tensor(out=sm[:], in0=Ps[:], in1=decinT[:], op=ALU.mult),
                                 reads=[Ps, decinT], writes=[sm])
                            for h in range(4):
                                Pr = pf()

                                def fn(e):
                                    e.matmul(Pr[:], lhsT=sm[:, h * 128:(h + 1) * 128], rhs=vtm[:, tc, h * 512:(h + 1) * 512],
                                             start=True, stop=False)
                                    e.matmul(Pr[:], lhsT=qsT[:, 2 * h, tsl], rhs=rst_b[:, 2 * h, :], start=False, stop=False)
                                    return e.matmul(Pr[:], lhsT=qsT[:, 2 * h + 1, tsl], rhs=rst_b[:, 2 * h + 1, :], start=False, stop=True)
                                k.op("pe", fn, reads=[sm, vtm, qsT, rst_b], writes=[Pr])
                                rms_rstd(Pr[:], Pr, 512, 1)
                                k.op("dve", lambda e: e.scalar_tensor_tensor(out=rg[:], in0=Pr[:], scalar=rstd[:, 1:2],
                                                                             in1=sg[:, tc, h * 512:(h + 1) * 512], op0=ALU.mult, op1=ALU.mult),
                                     reads=[Pr, rstd, sg], writes=[rg])
                                Pt = pb()

                                def fn(e):
                                    for j in range(4):
                                        ins = e.transpose(Pt[:, j * 128:(j + 1) * 128], rg[:, j * 128:(j + 1) * 128], ident_b[:])
                                    return ins
                                k.op("pe", fn, reads=[rg, ident_b], writes=[Pt])
                                k.op("act", lambda e: e.copy(out=retT[:, 4 * h:4 * h + 4, tsl],
                                                             in_=Pt[:, 0:512].rearrange("p (j t) -> p j t", j=4)), reads=[Pt], writes=[retT])
                                for dc in range(2):
                                    Pu = pf()
                                    k.op("pe", lambda e: e.matmul(Pu[:], lhsT=ktm[:, tc, h * 256 + dc * 128:h * 256 + (dc + 1) * 128],
                                                                  rhs=vtm[:, tc, h * 512:(h + 1) * 512], start=True, stop=True),
                                         reads=[ktm, vtm], writes=[Pu])
                                    k.op("dve", lambda e: e.scalar_tensor_tensor(out=rst_f[:, 2 * h + dc, :], in0=rst_f[:, 2 * h + dc, :],
                                                                                 scalar=gC[h], in1=Pu[:], op0=ALU.mult, op1=ALU.add),
                                         reads=[rst_f, Pu], writes=[rst_f])
                                    k.op("act", lambda e: e.copy(out=rst_b[:, 2 * h + dc, :], in_=rst_f[:, 2 * h + dc, :]),
                                         reads=[rst_f], writes=[rst_b])
                        if dbg and b == 0 and gt == 0:
                            dbg_out("d_retT", lambda: retT[:], retT, [128, 16, 256])
                        def out_projT(blk_base, srcT, dst_f32, first):
                            for nb in range(2):
                                Pa = [pf(), pf()]
                                Ws = [wload(esm, wb, blk_base + nb * 2 + kh) for kh in range(2)]

                                def fn(e):
                                    for cc in range(4):
                                        for kh in range(2):
                                            for kc in range(8):
                                                ins = e.matmul(Pa[cc // 2][:, (cc % 2) * 256:(cc % 2 + 1) * 256],
                                                               lhsT=Ws[kh][:, kc, cc * 128:(cc + 1) * 128], rhs=srcT[:, kh * 8 + kc, :],
                                                               start=(kh == 0 and kc == 0), stop=(kh == 1 and kc == 7))
                                    return ins
                                k.op("pe", fn, reads=[Ws[0], Ws[1], srcT], writes=[Pa[0], Pa[1]])
                                for half in range(2):
                                    cc0 = nb * 4 + half * 2
                                    k.op("act", lambda e: e.copy(out=dst_f32[:, cc0:cc0 + 2, :],
                                                                 in_=Pa[half][:].rearrange("p (c t) -> p c t", c=2)),
                                         reads=[Pa[half]], writes=[dst_f32])
                        out_projT(28, retT, maT, True)

                        for bi in range(4):
                            W = wload(esm, wb, 12 + bi)
                            for tc in range(2):
                                P = pf()
                                tm_group(W, tc, P, 512)
                                k.op("act", lambda e: e.activation(out=sg[:, tc, bi * 512:(bi + 1) * 512], in_=P[:], func=AF.Silu),
                                     reads=[P], writes=[sg])
                        pend = [None]

                        def mk_s2(cc, ca, co):
                            def s2():
                                if cc < 16:
                                    k.op("act", lambda e: e.activation(out=co[:], in_=ca[:], func=AF.Silu), reads=[ca], writes=[co])
                                    Pt = pb()

                                    def fn(e):
                                        for tc in range(2):
                                            ins = e.transpose(Pt[:, tc * 128:(tc + 1) * 128], co[:, tc * 128:(tc + 1) * 128], ident_b[:])
                                        return ins
                                    k.op("pe", fn, reads=[co, ident_b], writes=[Pt])
                                    k.op("act", lambda e: e.copy(out=xs[:, :, cc * 128:(cc + 1) * 128],
                                                                 in_=Pt[:, 0:256].rearrange("p (a t) -> p a t", a=2)),
                                         reads=[Pt], writes=[xs])
                                elif cc < 24:
                                    g_ = cc - 16
                                    k.op("act", lambda e: e.activation(out=bmT[:, g_, :], in_=ca[:], func=AF.Silu), reads=[ca], writes=[bmT])
                                    Pt = pb()

                                    def fn(e):
                                        for tc in range(2):
                                            ins = e.transpose(Pt[:, tc * 128:(tc + 1) * 128], bmT[:, g_, tc * 128:(tc + 1) * 128], ident_b[:])
                                        return ins
                                    k.op("pe", fn, reads=[bmT, ident_b], writes=[Pt])
                                    k.op("act", lambda e: e.copy(out=bmtm[:, :, g_ * 128:(g_ + 1) * 128],
                                                                 in_=Pt[:, 0:256].rearrange("p (a t) -> p a t", a=2)),
                                         reads=[Pt], writes=[bmtm])
                                else:
                                    g_ = cc - 24
                                    k.op("act", lambda e: e.activation(out=cmT[:, g_, :], in_=ca[:], func=AF.Silu), reads=[ca], writes=[cmT])
                            return s2

                        for bi in range(8):
                            W = wload(esm, wb, 16 + bi)
                            for half in range(2):
                                P = pf()
                                fm_group(W, half * 2, P, 0)
                                fm_group(W, half * 2 + 1, P, 256)
                                for c2 in range(2):
                                    cc = bi * 4 + half * 2 + c2
                                    cr = craw[cc % 2]
                                    ca = cacc[cc % 2]
                                    co = cso[cc % 2]
                                    k.op("act", lambda e: e.copy(out=cr[:, 0:3], in_=halo[:, cc, :]), reads=[halo], writes=[cr])
                                    k.op("act", lambda e: e.copy(out=cr[:, 3:259], in_=P[:, c2 * 256:(c2 + 1) * 256]), reads=[P], writes=[cr])
                                    k.op("act", lambda e: e.copy(out=halo[:, cc, :], in_=cr[:, 256:259]), reads=[cr], writes=[halo])
                                    k.op("dve", lambda e: e.tensor_scalar(out=ca[:], in0=cr[:, 3:259], scalar1=cw[:, cc, 3:4],
                                                                          scalar2=cb[:, cc:cc + 1], op0=ALU.mult, op1=ALU.add),
                                         reads=[cr, cw, cb], writes=[ca])
                                    for kk in range(3):
                                        k.op("dve", (lambda kk: (lambda e: e.scalar_tensor_tensor(
                                            out=ca[:], in0=cr[:, kk:kk + 256], scalar=cw[:, cc, kk:kk + 1], in1=ca[:],
                                            op0=ALU.mult, op1=ALU.add)))(kk), reads=[cr, cw, ca], writes=[ca])
                                    if pend[0] is not None:
                                        pend[0]()
                                    pend[0] = mk_s2(cc, ca, co)
                        pend[0]()
                        pend[0] = None
                        for tc in range(2):
                            P = pf()
                            tm_group(wdt, tc, P, 32)
                            k.op("dve", lambda e: e.tensor_tensor(out=dtt[:, tc, :], in0=P[:, 0:32], in1=dtb_bc[:], op=ALU.add),
                                 reads=[P, dtb_bc], writes=[dtt])
                        k.op("act", lambda e: e.activation(out=dtt[:], in_=dtt[:], func=AF.Exp), reads=[dtt], writes=[dtt])
                        k.op("act", lambda e: e.activation(out=dtt[:], in_=dtt[:], func=AF.Ln, bias=oneb[:]), reads=[dtt, oneb], writes=[dtt])
                        if dbg and b == 0 and gt == 0:
                            dbg_out("d_xs", lambda: xs[:], xs, [128, 2, 2048])
                            dbg_out("d_dt", lambda: dtt[:], dtt, [128, 2, 32])
                            dbg_out("d_bmT", lambda: bmT[:], bmT, [128, 8, 256])
                            dbg_out("d_cmT", lambda: cmT[:], cmT, [128, 8, 256])
                        for tc in range(2):
                            tsl = slice(tc * 128, (tc + 1) * 128)
                            k.op("dve", lambda e: e.tensor_tensor(out=adt[:], in0=dtt[:, tc, :], in1=a_bc[:], op=ALU.mult),
                                 reads=[dtt, a_bc], writes=[adt])
                            P = pf()
                            k.op("pe", lambda e: e.matmul(P[:, 0:32], lhsT=tri_f[:], rhs=adt[:], start=True, stop=True),
                                 reads=[tri_f, adt], writes=[P])
                            P2 = pf()
                            k.op("pe", lambda e: e.matmul(P2[:, 0:32], lhsT=ones_f[:], rhs=adt[:], start=True, stop=True),
                                 reads=[ones_f, adt], writes=[P2])
                            k.op("act", lambda e: e.copy(out=acs[:], in_=P[:, 0:32]), reads=[P], writes=[acs])
                            k.op("dve", lambda e: e.tensor_scalar(out=nacs[:], in0=P[:, 0:32], scalar1=-1.0, scalar2=None, op0=ALU.mult),
                                 reads=[P], writes=[nacs])
                            k.op("act", lambda e: e.activation(out=eacs[:], in_=P[:, 0:32], func=AF.Exp), reads=[P], writes=[eacs])
                            k.op("act", lambda e: e.activation(out=etot[:], in_=P2[:, 0:32], func=AF.Exp), reads=[P2], writes=[etot])
                            k.op("dve", lambda e: e.tensor_tensor(out=dS[:], in0=P2[:, 0:32], in1=acs[:], op=ALU.subtract),
                                 reads=[P2, acs], writes=[dS])
                            k.op("act", lambda e: e.activation(out=dS[:], in_=dS[:], func=AF.Exp), reads=[dS], writes=[dS])
                            for hf in range(2):
                                Pc = pf()

                                def fn(e):
                                    for gg in range(4):
                                        g_ = hf * 4 + gg
                                        ins = e.matmul(Pc[:, gg * 128:(gg + 1) * 128], lhsT=bmT[:, g_, tsl], rhs=cmT[:, g_, tsl], start=True, stop=True)
                                    return ins
                                k.op("pe", fn, reads=[bmT, cmT], writes=[Pc])
                                k.op("act", lambda e: e.copy(out=cbT[:, hf * 4:(hf + 1) * 4, :], in_=Pc[:].rearrange("p (g l) -> p g l", g=4)),
                                     reads=[Pc], writes=[cbT])
                            def stA(g_):
                                X, sgm, M, xd, xS = Xg[g_ % 2], seg[g_ % 2], Mg[g_ % 2], xdt[g_ % 2], xdS[g_ % 2]
                                hs = slice(4 * g_, 4 * g_ + 4)
                                cs_ = slice(g_ * 256, (g_ + 1) * 256)
                                k.op("dve", lambda e: e.tensor_tensor(out=X[:], in0=tri_f[:].unsqueeze(1).broadcast_to([128, 4, 128]),
                                                                      in1=adt[:, hs].unsqueeze(2).broadcast_to([128, 4, 128]), op=ALU.mult),
                                     reads=[tri_f, adt], writes=[X])
                                Pa = pf()

                                def fn(e):
                                    e.matmul(Pa[:], lhsT=ones_f[:], rhs=X[:].rearrange("p a l -> p (a l)"), start=True, stop=False)
                                    return e.matmul(Pa[:], lhsT=ident_f[:], rhs=nm_f[:], start=False, stop=True)
                                k.op("pe", fn, reads=[ones_f, X, ident_f, nm_f], writes=[Pa])
                                for hh in range(4):
                                    k.op("act", lambda e: e.activation(out=sgm[:, hh, :], in_=Pa[:, hh * 128:(hh + 1) * 128], func=AF.Exp,
                                                                       bias=nacs[:, 4 * g_ + hh:4 * g_ + hh + 1]),
                                         reads=[Pa, nacs], writes=[sgm])
                                k.op("dve", lambda e: e.tensor_tensor(out=M[:], in0=sgm[:], in1=cbT[:, g_, :].unsqueeze(1).broadcast_to([128, 4, 128]),
                                                                      op=ALU.mult), reads=[sgm, cbT], writes=[M])
                                k.op("dve", lambda e: e.tensor_tensor(out=xd[:].rearrange("p (h q) -> p h q", h=4),
                                                                      in0=xs[:, tc, cs_].rearrange("p (h q) -> p h q", h=4),
                                                                      in1=dtt[:, tc, hs].unsqueeze(2).broadcast_to([128, 4, 64]), op=ALU.mult),
                                     reads=[xs, dtt], writes=[xd])
                                k.op("dve", lambda e: e.tensor_tensor(out=xS[:].rearrange("p (h q) -> p h q", h=4),
                                                                      in0=xd[:].rearrange("p (h q) -> p h q", h=4),
                                                                      in1=dS[:, hs].unsqueeze(2).broadcast_to([128, 4, 64]), op=ALU.mult),
                                     reads=[xd, dS], writes=[xS])

                            def stB(g_):
                                yt1, yt2, ynb = yt1_[g_ % 2], yt2_[g_ % 2], ynb_[g_ % 2]
                                M, xd = Mg[g_ % 2], xdt[g_ % 2]
                                sq_, rs_, sj_ = ssq2[g_ % 2], rstd2[g_ % 2], sqj2[g_ % 2]
                                hs = slice(4 * g_, 4 * g_ + 4)
                                cs_ = slice(g_ * 256, (g_ + 1) * 256)
                                Py = pf()

                                def fn(e):
                                    for hh in range(4):
                                        e.matmul(Py[:, hh * 64:(hh + 1) * 64], lhsT=M[:, hh, :], rhs=xd[:, hh * 64:(hh + 1) * 64], start=True, stop=True)
                                    return e.matmul(Py[:, 256:512], lhsT=cmT[:, g_, tsl], rhs=sst_b[g_][:], start=True, stop=True)
                                k.op("pe", fn, reads=[M, xd, cmT, sst_b[g_]], writes=[Py])
                                k.op("dve", lambda e: e.tensor_tensor(out=yt1[:].rearrange("p (h q) -> p h q", h=4),
                                                                      in0=Py[:, 256:512].rearrange("p (h q) -> p h q", h=4),
                                                                      in1=eacs[:, hs].unsqueeze(2).broadcast_to([128, 4, 64]), op=ALU.mult),
                                     reads=[Py, eacs], writes=[yt1])
                                k.op("dve", lambda e: e.tensor_tensor(out=yt1[:], in0=yt1[:], in1=Py[:, 0:256], op=ALU.add),
                                     reads=[yt1, Py], writes=[yt1])
                                k.op("dve", lambda e: e.tensor_tensor(out=yt2[:].rearrange("p (h q) -> p h q", h=4),
                                                                      in0=xs[:, tc, cs_].rearrange("p (h q) -> p h q", h=4),
                                                                      in1=dsk_bc[:, hs].unsqueeze(2).broadcast_to([128, 4, 64]), op=ALU.mult),
                                     reads=[xs, dsk_bc], writes=[yt2])
                                k.op("dve", lambda e: e.tensor_tensor(out=yt1[:], in0=yt1[:], in1=yt2[:], op=ALU.add),
                                     reads=[yt1, yt2], writes=[yt1])
                                k.op("dve", lambda e: e.tensor_tensor(out=yt1[:], in0=yt1[:], in1=sg[:, tc, cs_], op=ALU.mult),
                                     reads=[yt1, sg], writes=[yt1])
                                k.op("dve", lambda e: e.memset(sq_[:], 0.0), writes=[sq_])
                                k.op("act", lambda e: e.activation(out=sj_[:], in_=yt1[:], func=AF.Square, accum_out=sq_[:]),
                                     reads=[yt1, sq_], writes=[sj_, sq_])
                                k.op("act", lambda e: e.activation(out=rs_[:], in_=sq_[:], func=AF.Ln, scale=1.0 / 256, bias=epsb[:]),
                                     reads=[sq_, epsb], writes=[rs_])
                                k.op("act", lambda e: e.activation(out=rs_[:], in_=rs_[:], func=AF.Exp, scale=-0.5), reads=[rs_], writes=[rs_])
                                k.op("dve", lambda e: e.scalar_tensor_tensor(out=ynb[:], in0=yt1[:], scalar=rs_[:, 0:1], in1=ssmn_bc[:, cs_],
                                                                             op0=ALU.mult, op1=ALU.mult), reads=[yt1, rs_, ssmn_bc], writes=[ynb])
                                Pt = pb()

                                def fn(e):
                                    for j in range(2):
                                        ins = e.transpose(Pt[:, j * 128:(j + 1) * 128], ynb[:, j * 128:(j + 1) * 128], ident_b[:])
                                    return ins
                                k.op("pe", fn, reads=[ynb, ident_b], writes=[Pt])
                                k.op("act", lambda e: e.copy(out=yT[:, 2 * g_:2 * g_ + 2, tsl], in_=Pt[:, 0:256].rearrange("p (j t) -> p j t", j=2)),
                                     reads=[Pt], writes=[yT])

                            def stC(g_):
                                xS = xdS[g_ % 2]
                                hs = slice(4 * g_, 4 * g_ + 4)
                                Pu = pf()
                                k.op("pe", lambda e: e.matmul(Pu[:, 0:256], lhsT=bmtm[:, tc, g_ * 128:(g_ + 1) * 128], rhs=xS[:], start=True, stop=True),
                                     reads=[bmtm, xS], writes=[Pu])
                                k.op("dve", lambda e: e.tensor_tensor(out=sst_f[g_][:].rearrange("p (h q) -> p h q", h=4),
                                                                      in0=sst_f[g_][:].rearrange("p (h q) -> p h q", h=4),
                                                                      in1=etot[:, hs].unsqueeze(2).broadcast_to([128, 4, 64]), op=ALU.mult),
                                     reads=[sst_f[g_], etot], writes=[sst_f[g_]])
                                k.op("dve", lambda e: e.tensor_tensor(out=sst_f[g_][:], in0=sst_f[g_][:], in1=Pu[:, 0:256], op=ALU.add),
                                     reads=[sst_f[g_], Pu], writes=[sst_f[g_]])
                                k.op("act", lambda e: e.copy(out=sst_b[g_][:], in_=sst_f[g_][:]), reads=[sst_f[g_]], writes=[sst_b[g_]])

                            stA(0)
                            for g_ in range(8):
                                if g_ < 7:
                                    stA(g_ + 1)
                                stB(g_)
                                stC(g_)
                        if dbg and b == 0 and gt == 0:
                            dbg_out("d_yT", lambda: yT[:], yT, [128, 16, 256])
                        for (c0, dstT) in ((24, gaT), (26, gbT)):
                            for bi in range(2):
                                W = wload(esm, wb, c0 + bi)
                                for half in range(2):
                                    P = pf()
                                    fm_group(W, half * 2, P, 0)
                                    fm_group(W, half * 2 + 1, P, 256)
                                    cc0 = bi * 4 + half * 2
                                    k.op("act", lambda e: e.activation(out=dstT[:, cc0:cc0 + 2, :], in_=P[:].rearrange("p (c t) -> p c t", c=2),
                                                                       func=AF.Sigmoid), reads=[P], writes=[dstT])
                        k.op("dve", lambda e: e.tensor_tensor(out=maT[:], in0=maT[:], in1=gaT[:], op=ALU.mult), reads=[maT, gaT], writes=[maT])
                        for nb in range(2):
                            Pa = [pf(), pf()]
                            Ws = [wload(esm, wb, 32 + nb * 2 + kh) for kh in range(2)]

                            def fn(e):
                                for cc in range(4):
                                    for kh in range(2):
                                        for kc in range(8):
                                            ins = e.matmul(Pa[cc // 2][:, (cc % 2) * 256:(cc % 2 + 1) * 256],
                                                           lhsT=Ws[kh][:, kc, cc * 128:(cc + 1) * 128], rhs=yT[:, kh * 8 + kc, :],
                                                           start=(kh == 0 and kc == 0), stop=(kh == 1 and kc == 7))
                                return ins
                            k.op("pe", fn, reads=[Ws[0], Ws[1], yT], writes=[Pa[0], Pa[1]])
                            for half in range(2):
                                cc0 = nb * 4 + half * 2
                                k.op("dve", lambda e: e.tensor_tensor(out=rA[:].rearrange("p (c t) -> p c t", c=2),
                                                                      in0=Pa[half][:].rearrange("p (c t) -> p c t", c=2),
                                                                      in1=gbT[:, cc0:cc0 + 2, :], op=ALU.mult), reads=[Pa[half], gbT], writes=[rA])
                                k.op("dve", lambda e: e.tensor_tensor(out=mgT[:, cc0:cc0 + 2, :], in0=rA[:].rearrange("p (c t) -> p c t", c=2),
                                                                      in1=maT[:, cc0:cc0 + 2, :], op=ALU.add), reads=[rA, maT], writes=[mgT])
                        for nb in range(2):
                            W = wload(esm, wb, 36 + nb)
                            for tc in range(2):
                                P = pf()

                                def fn(e):
                                    for kc in range(8):
                                        ins = e.matmul(P[:], lhsT=mgT[:, kc, tc * 128:(tc + 1) * 128], rhs=W[:, kc, :], start=(kc == 0), stop=(kc == 7))
                                    return ins
                                k.op("pe", fn, reads=[mgT, W], writes=[P])
                                k.op("dve", lambda e: e.tensor_tensor(out=otmp[:], in0=P[:], in1=modm[2][:, nb * 512:(nb + 1) * 512], op=ALU.mult),
                                     reads=[P, modm[2]], writes=[otmp])
                                k.op("dve", lambda e: e.tensor_tensor(out=acc[:, tb0 + tc, nb * 512:(nb + 1) * 512],
                                                                      in0=acc[:, tb0 + tc, nb * 512:(nb + 1) * 512], in1=otmp[:], op=ALU.add),
                                     reads=[acc, otmp], writes=[acc])
                        if dbg:
                            k.dma("sp", lambda e: e.dma_start(out=dbg_d["d_x1"][b, tok0:tok0 + 256, :].rearrange("(tc p) d -> p tc d", p=128),
                                                              in_=acc[:, tb0:tb0 + 2, :]), reads=[acc])
                    if u < NU - 1:
                        k.dma("sp", lambda e: e.dma_start(out=srf_d, in_=rst_f[:].rearrange("p a b -> p (a b)")), reads=[rst_f])
                        for i in range(8):
                            k.dma("sp", lambda e: e.dma_start(out=ssf_d[:, i * 256:(i + 1) * 256], in_=sst_f[i][:]), reads=[sst_f[i]])
                        k.dma("sp", lambda e: e.dma_start(out=shl_d, in_=halo[:].rearrange("p a b -> p (a b)")), reads=[halo])
                    k.barrier()
                if os.environ.get("MK_STOP") == "mixer":
                    k.finish()
                    return nc
                with ExitStack() as ese:
                    hold["htmp"] = k.sb("htmp", [128, D], F32, ese)
                    h2T = k.sb("h2T", [128, 8, U], BF16, ese)
                    actT = k.sb("actT", [128, 8, U], BF16, ese)
                    wgu = [k.sb("wgu%d" % i, [128, 8, 2048], BF16, ese) for i in range(1)]
                    wdn = [k.sb("wdn%d" % i, [128, 8, 1024], BF16, ese) for i in range(1)]
                    bdn = [k.sb("bdn%d" % i, [1, 1024], BF16, ese) for i in range(2)]
                    wr_b = k.sb("wr_b", [128, 8, NEXP], BF16, ese)
                    br_b = k.sb("br_b", [1, NEXP], BF16, ese)
                    bgu = k.sb("bgu", [128, NEXP, 16], F32, ese)
                    modf = [k.sb("modf%d" % i, [128, D], F32, ese) for i in range(3)]
                    k.dma("pool", lambda e: e.dma_start(out=wr_b[:], in_=wr_d.rearrange("(kc p) e -> p kc e", p=128)), writes=[wr_b])
                    k.dma("pool", lambda e: e.dma_start(out=br_b[:], in_=br_d), writes=[br_b])
                    k.dma("sp", lambda e: e.dma_start(out=bgu[:], in_=bgu_d), writes=[bgu])
                    with ExitStack() as esi:
                        condbc = k.sb("condbc", [128, 8, 128], F32, esi)
                        wst = k.sb("wst", [128, 8, 256], F32, esi)
                        nrm_bc = k.sb("nrm_bc", [128, D], F32, esi)
                        k.op("dve", lambda e: e.tensor_copy(out=condbc[:], in_=cs[:].unsqueeze(2).broadcast_to([128, 8, 128])),
                             reads=[cs], writes=[condbc])
                        compute_mod(b, 3, modf, condbc, wst)
                        k.dma("sp", lambda e: e.dma_start(out=nrm_bc[:], in_=bc(nffn_d)), writes=[nrm_bc])
                        k.op("dve", lambda e: e.scalar_tensor_tensor(out=modf[1][:], in0=modf[1][:], scalar=1.0, in1=nrm_bc[:],
                                                                     op0=ALU.add, op1=ALU.mult), reads=[modf[1], nrm_bc], writes=[modf[1]])
                        k.barrier()
                    gw = k.sb("gw", [128, NTB, NEXP], F32, ese)
                    lg = k.sb("lg", [128, NEXP], F32, ese)
                    top8 = k.sb("top8", [128, 8], F32, ese)
                    negm = k.sb("negm", [128, 1], F32, ese)
                    msk = k.sb("msk", [128, NEXP], F32, ese)
                    rsum = k.sb("rsum", [128, 1], F32, ese)
                    gt_ = [k.sb("gt%d" % i, [128, TGW], F32, ese) for i in range(2)]
                    ut_ = [k.sb("ut%d" % i, [128, TGW], F32, ese) for i in range(2)]
                    sgt = [k.sb("sgt%d" % i, [128, TGW], F32, ese) for i in range(2)]
                    mtmp = [k.sb("mtmp%d" % i, [128, 512], F32, ese) for i in range(2)]
                    mstg = [k.sb("mstg%d" % i, [128, 8, 256], F32, ese) for i in range(2)]
                    st["ms"] = 0
                    nfin_bc = k.sb("nfin_bc", [128, D], F32, ese)
                    k.dma("sp", lambda e: e.dma_start(out=nfin_bc[:], in_=bc(nfin_d)), writes=[nfin_bc])
                    print("sbuf remaining (moe)", nc.sbuf_bytes_remaining, flush=True)

                    for tb in range(NTB):
                        norm_mod_T(tb, modf[1], modf[0], h2T, tb * 128)
                        P = pf()

                        def fn(e):
                            for kc in range(8):
                                e.matmul(P[:, 0:NEXP], lhsT=h2T[:, kc, tb * 128:(tb + 1) * 128], rhs=wr_b[:, kc, :], start=(kc == 0), stop=False)
                            return e.matmul(P[:, 0:NEXP], lhsT=ones_b[0:1, :], rhs=br_b[0:1, :], start=False, stop=True)
                        k.op("pe", fn, reads=[h2T, wr_b, ones_b, br_b], writes=[P])
                        k.op("act", lambda e: e.copy(out=lg[:], in_=P[:, 0:NEXP]), reads=[P], writes=[lg])
                        k.op("dve", lambda e: e.max(out=top8[:], in_=lg[:]), reads=[lg], writes=[top8])
                        k.op("dve", lambda e: e.tensor_scalar(out=negm[:], in0=top8[:, 0:1], scalar1=-1.0, scalar2=None, op0=ALU.mult),
                             reads=[top8], writes=[negm])
                        k.op("dve", lambda e: e.tensor_scalar(out=msk[:], in0=lg[:], scalar1=top8[:, 3:4], scalar2=None, op0=ALU.is_ge),
                             reads=[lg, top8], writes=[msk])
                        k.op("act", lambda e: e.activation(out=lg[:], in_=lg[:], func=AF.Exp, bias=negm[:]), reads=[lg, negm], writes=[lg])
                        k.op("dve", lambda e: e.tensor_tensor(out=lg[:], in0=lg[:], in1=msk[:], op=ALU.mult), reads=[lg, msk], writes=[lg])
                        k.op("dve", lambda e: e.tensor_reduce(out=rsum[:], in_=lg[:], axis=AX.X, op=ALU.add), reads=[lg], writes=[rsum])
                        k.op("dve", lambda e: e.reciprocal(out=rsum[:], in_=rsum[:]), reads=[rsum], writes=[rsum])
                        k.op("dve", lambda e: e.tensor_scalar(out=gw[:, tb, :], in0=lg[:], scalar1=rsum[:, 0:1], scalar2=None, op0=ALU.mult),
                             reads=[lg, rsum], writes=[gw])
                    if dbg and b == 0 and u == 0:
                        k.dma("sp", lambda e: e.dma_start(out=dbg_d["d_gw"], in_=gw[:]), reads=[gw])

                    for ex in range(NEXP):
                        Wg = wgu[0]
                        Wd = wdn[0]
                        Bd = bdn[ex % 2]
                        for q8 in range(8):
                            sg_ = mstg[st["ms"] % 2]
                            st["ms"] += 1
                            k.dma("sp", lambda e: e.dma_start(out=sg_[:], in_=wgu_d[ex, :, q8 * 256:(q8 + 1) * 256].rearrange(
                                "(kc p) c -> p kc c", p=128)), writes=[sg_])
                            k.op("act", lambda e: e.copy(out=Wg[:, :, q8 * 256:(q8 + 1) * 256], in_=sg_[:]), reads=[sg_], writes=[Wg])
                        for q8 in range(4):
                            sg_ = mstg[st["ms"] % 2]
                            st["ms"] += 1
                            k.dma("sp", lambda e: e.dma_start(out=sg_[:], in_=wd_d[ex, :, q8 * 256:(q8 + 1) * 256].rearrange(
                                "(kc p) c -> p kc c", p=128)), writes=[sg_])
                            k.op("dve", lambda e: e.tensor_tensor(out=Wd[:, :, q8 * 256:(q8 + 1) * 256], in0=sg_[:],
                                                                  in1=modf[2][:, q8 * 256:(q8 + 1) * 256].unsqueeze(1).broadcast_to([128, 8, 256]),
                                                                  op=ALU.mult), reads=[sg_, modf[2]], writes=[Wd])
                        k.dma("pool", lambda e: e.dma_start(out=Bd[:], in_=bd_d[ex:ex + 1, :]), writes=[Bd])
                        k.op("dve", lambda e: e.tensor_tensor(out=Bd[:], in0=Bd[:], in1=modf[2][0:1, :], op=ALU.mult), reads=[Bd, modf[2]], writes=[Bd])
                        for fc in range(8):
                            for tg in range(NTG):
                                tgs = slice(tg * TGW, (tg + 1) * TGW)
                                Pg = pf()
                                Pu = pf()

                                def fn(e):
                                    for kc in range(8):
                                        e.matmul(Pg[:, 0:TGW], lhsT=Wg[:, kc, fc * 128:(fc + 1) * 128], rhs=h2T[:, kc, tgs], start=(kc == 0), stop=(kc == 7))
                                    for kc in range(8):
                                        ins = e.matmul(Pu[:, 0:TGW], lhsT=Wg[:, kc, 1024 + fc * 128:1024 + (fc + 1) * 128], rhs=h2T[:, kc, tgs],
                                                       start=(kc == 0), stop=(kc == 7))
                                    return ins
                                k.op("pe", fn, reads=[Wg, h2T], writes=[Pg, Pu])
                                i2 = (fc * NTG + tg) % 2
                                G, Uu, Sg = gt_[i2], ut_[i2], sgt[i2]
                                k.op("dve", lambda e: e.tensor_scalar(out=G[:], in0=Pg[:, 0:TGW], scalar1=bgu[:, ex, fc:fc + 1], scalar2=7.0,
                                                                      op0=ALU.add, op1=ALU.min), reads=[Pg, bgu], writes=[G])
                                k.op("act", lambda e: e.activation(out=Uu[:], in_=Pu[:, 0:TGW], func=AF.Identity, bias=bgu[:, ex, 8 + fc:9 + fc]),
                                     reads=[Pu, bgu], writes=[Uu])
                                k.op("act", lambda e: e.activation(out=Sg[:], in_=G[:], func=AF.Sigmoid, scale=1.702), reads=[G], writes=[Sg])
                                k.op("dve", lambda e: e.tensor_scalar(out=Uu[:], in0=Uu[:], scalar1=-7.0, scalar2=7.0, op0=ALU.max, op1=ALU.min),
                                     reads=[Uu], writes=[Uu])
                                k.op("dve", lambda e: e.tensor_tensor(out=G[:], in0=G[:], in1=Sg[:], op=ALU.mult), reads=[G, Sg], writes=[G])
                                k.op("dve", lambda e: e.scalar_tensor_tensor(out=actT[:, fc, tgs], in0=Uu[:], scalar=1.0, in1=G[:],
                                                                             op0=ALU.add, op1=ALU.mult), reads=[Uu, G], writes=[actT])
                        for tb in range(NTB):
                            for nb in range(2):
                                P = pf()

                                def fn(e):
                                    for fc in range(8):
                                        e.matmul(P[:], lhsT=actT[:, fc, tb * 128:(tb + 1) * 128], rhs=Wd[:, fc, nb * 512:(nb + 1) * 512],
                                                 start=(fc == 0), stop=False)
                                    return e.matmul(P[:], lhsT=ones_b[0:1, :], rhs=Bd[0:1, nb * 512:(nb + 1) * 512], start=False, stop=True)
                                k.op("pe", fn, reads=[actT, Wd, ones_b, Bd], writes=[P])
                                k.op("dve", lambda e: e.scalar_tensor_tensor(out=acc[:, tb, nb * 512:(nb + 1) * 512], in0=P[:],
                                                                             scalar=gw[:, tb, ex:ex + 1],
                                                                             in1=acc[:, tb, nb * 512:(nb + 1) * 512], op0=ALU.mult, op1=ALU.add),
                                     reads=[P, gw, acc], writes=[acc])
                    for tb in range(NTB):
                        rms_rstd(acc[:, tb, :], acc, D, 3)
                        k.op("dve", lambda e: e.scalar_tensor_tensor(out=acc[:, tb, :], in0=acc[:, tb, :], scalar=rstd[:, 3:4], in1=nfin_bc[:],
                                                                     op0=ALU.mult, op1=ALU.mult), reads=[acc, rstd, nfin_bc], writes=[acc])
                    k.dma("sp", lambda e: e.dma_start(out=out_d[b, u * U:(u + 1) * U, :].rearrange("(tb p) d -> p tb d", p=128), in_=acc[:]),
                          reads=[acc])
                    k.barrier()
        k.finish()
        print("instr counts", k.ninstr, flush=True)
    return nc


def prep_inputs(inp, NSEQ, S, ncores):
    consts, gC = host_consts(S)
    f = lambda a: np.ascontiguousarray(a, dtype=np.float32)
    shared = {
        "w_ada": f(inp["w_ada"][0]), "b_ada": f(inp["b_ada"][0][None, :]),
        "norm_mix": f(inp["norm_mix"][0][None, :]), "norm_ffn": f(inp["norm_ffn"][0][None, :]),
        "norm_final": f(inp["norm_final"][None, :]), "ssm_norm": f(inp["ssm_norm"][0][None, :]),
        "dt_bias": f(inp["dt_bias"][0][None, :]), "a_log": f(inp["a_log"][0][None, :]), "d_skip": f(inp["d_skip"][0][None, :]),
        "w_in": f(inp["w_in"][0]),
        "cw": f(inp["conv_w"][0].reshape(4, 32, 128).transpose(2, 1, 0)),
        "cb": f(inp["conv_b"][0].reshape(32, 128).T),
        "w_ret_out": f(inp["w_ret_out"][0]), "w_ssm_out": f(inp["w_ssm_out"][0]), "w_out": f(inp["w_out"][0]),
        "w_router": f(inp["w_router"][0]), "b_router": f(inp["b_router"][0][None, :]),
        "w_gate_up": f(inp["w_gate_up"][0]),
        "bgu": f(inp["b_gate_up"][0].reshape(NEXP, 16, 128).transpose(2, 0, 1)),
        "w_down": f(inp["w_down"][0]), "b_down": f(inp["b_down"][0]),
    }
    shared.update(consts)
    x = np.asarray(inp["x"], dtype=np.float32)
    c = np.asarray(inp["c"], dtype=np.float32)
    maps = []
    for i in range(ncores):
        m = dict(shared)
        m["x"] = np.ascontiguousarray(x[i * NSEQ:(i + 1) * NSEQ])
        m["cl"] = f(c[i * NSEQ:(i + 1) * NSEQ].reshape(NSEQ, 8, 128).transpose(0, 2, 1))
        maps.append(m)
    return maps, gC


def kernel(**inputs):
    B, S, _ = inputs["x"].shape
    ncores = 8
    NSEQ = B // ncores
    maps, gC = prep_inputs(inputs, NSEQ, S, ncores)
    nc = build(NSEQ, S, gC)
    res = run_bass_kernel_spmd(nc, maps, core_ids=list(range(ncores)))
    out = np.concatenate([np.asarray(r["out"]) for r in res.results], axis=0)
    return out.astype(np.float32)
```

```python
import os
import numpy as np
import concourse.bass as bass
import concourse.mybir as mybir
from concourse.bass_utils import run_bass_kernel_spmd
from contextlib import ExitStack

F32 = mybir.dt.float32
BF16 = mybir.dt.bfloat16
AF = mybir.ActivationFunctionType
ALU = mybir.AluOpType
AX = mybir.AxisListType

NSLOT = 6
D = 1024
DIN = 14368
NEXP = 32
EPS = 1e-6


class Tl:
    def __init__(self, h, name=""):
        self.h = h
        self.name = name
        self.w = None
        self.r = []

    def __getitem__(self, k):
        return self.h[k]


class V:
    def __init__(self, tl, ap):
        self.tl = tl
        self.ap = ap

    def __getitem__(self, k):
        return self.ap[k]


def _norm(ts):
    return [getattr(t, "tl", t) for t in ts]


class Sched:
    ENG = ("pe", "act", "dve", "pool", "sp")

    def __init__(self, nc, es):
        self.nc = nc
        self.es = es
        self.engobj = {"pe": nc.tensor, "act": nc.scalar, "dve": nc.vector, "pool": nc.gpsimd, "sp": nc.sync}
        self.sems = {}
        self.cnt = {}
        for e in ("pe", "act", "dve", "pool"):
            self.sems[e] = es.enter_context(nc.semaphore("s_" + e))
            self.cnt[e] = 0
        self.dq = {}
        for q in ("sp", "pool"):
            self.dq[q] = 0
            for s in range(NSLOT):
                key = "d_%s_%d" % (q, s)
                self.sems[key] = es.enter_context(nc.semaphore(key))
                self.cnt[key] = 0
        self.waited = {e: {} for e in self.ENG}
        self.ninstr = {e: 0 for e in self.ENG}

    def sb(self, name, shape, dt, es=None):
        es = es or self.es
        self.uid = getattr(self, "uid", 0) + 1
        name = "sb%d_%s" % (self.uid, name)
        return Tl(es.enter_context(self.nc.sbuf_tensor(name, list(shape), dt)), name)

    def ps(self, name, shape, dt=F32):
        return Tl(self.es.enter_context(self.nc.psum_tensor(name, list(shape), dt)), name)

    def _deps(self, reads, writes):
        deps = []
        for t in reads:
            if t.w is not None:
                deps.append(t.w)
        for t in writes:
            if t.w is not None:
                deps.append(t.w)
            deps.extend(t.r)
        return deps

    def _waits(self, eng, deps):
        need = {}
        for (key, c) in deps:
            if eng == "pe" and key == "pe":
                continue
            if self.waited[eng].get(key, 0) < c and need.get(key, 0) < c:
                need[key] = c
        out = []
        for key, c in need.items():
            self.waited[eng][key] = c
            out.append((self.sems[key], c))
        return out

    def _issue(self, engname, waits, fn, sem, inc):
        eng = self.engobj[engname]
        for (s, c) in waits:
            eng.wait_ge(s, c)
        if fn is not None:
            fn(eng).then_inc(sem, inc)
        self.ninstr[engname] += 1

    def op(self, eng, fn, reads=(), writes=()):
        reads, writes = _norm(reads), _norm(writes)
        waits = self._waits(eng, self._deps(reads, writes))
        self.cnt[eng] += 1
        c = self.cnt[eng]
        self._issue(eng, waits, fn, self.sems[eng], 1)
        for t in reads:
            t.r.append((eng, c))
            if len(t.r) > 64:
                t.r = t.r[-48:] if False else self._compact(t.r)
        for t in writes:
            t.w = (eng, c)
            t.r = []
        return c

    @staticmethod
    def _compact(r):
        best = {}
        for (k, c) in r:
            if best.get(k, 0) < c:
                best[k] = c
        return list(best.items())

    def dma(self, q, fn, reads=(), writes=()):
        reads, writes = _norm(reads), _norm(writes)
        slot = self.dq[q] % NSLOT
        self.dq[q] += 1
        key = "d_%s_%d" % (q, slot)
        deps = self._deps(reads, writes)
        if self.cnt[key] > 0:
            deps.append((key, self.cnt[key]))
        waits = self._waits(q, deps)
        self.cnt[key] += 16
        c = self.cnt[key]
        self._issue(q, waits, fn, self.sems[key], 16)
        for t in reads:
            t.r.append((key, c))
        for t in writes:
            t.w = (key, c)
            t.r = []
        return c

    def barrier(self):
        for e in self.ENG:
            deps = [(k, c) for k, c in self.cnt.items() if c > 0]
            waits = self._waits(e, deps)
            if waits:
                self._issue(e, waits, None, None, 0)

    def finish(self):
        deps = [(k, c) for k, c in self.cnt.items() if c > 0]
        self._issue("sp", self._waits("sp", deps), None, None, 0)


def host_consts(S):
    nt = S // 256
    half = 128
    inv_freq = (10000.0 ** (-np.arange(half, dtype=np.float32) / half)).astype(np.float32)
    pos = np.arange(S, dtype=np.float32)
    ang = (pos[None, :] * inv_freq[:, None]).astype(np.float32)
    cos = np.cos(ang).astype(np.float32)
    sin = np.sin(ang).astype(np.float32)
    rope = np.zeros((nt, 128, 1024), np.float32)
    for t in range(nt):
        c = cos[:, t * 256:(t + 1) * 256]
        s = sin[:, t * 256:(t + 1) * 256]
        rope[t] = np.concatenate([c, s, s, c], axis=1)
    gam = 1.0 - 2.0 ** (-5.0 - np.arange(4, dtype=np.float64))
    idx = np.arange(128, dtype=np.float64)
    decinT = np.zeros((128, 512), np.float64)
    decq = np.zeros((128, 512), np.float64)
    deck = np.zeros((128, 4), np.float64)
    for h in range(4):
        rel = idx[None, :] - idx[:, None]
        decinT[:, h * 128:(h + 1) * 128] = np.where(rel >= 0, gam[h] ** np.maximum(rel, 0), 0.0) / 16.0
        decq[:, h * 128:(h + 1) * 128] = (gam[h] ** (idx + 1.0))[None, :]
        deck[:, h] = gam[h] ** (127.0 - idx) / 16.0
    gC = [float(gam[h] ** 128.0) for h in range(4)]
    tri = (idx[:, None] <= idx[None, :]).astype(np.float32)
    nm1 = np.where(idx[None, :] < idx[:, None], -30000.0, 0.0)
    nm = np.tile(nm1, (1, 4)).astype(np.float32)
    return dict(rope=rope, decinT=decinT.astype(np.float32), decq=decq.astype(np.float32),
                deck=deck.astype(np.float32), tri=tri, nm=nm,
                identf=np.eye(128, dtype=np.float32)), gC


def build(NSEQ, S, gC, dbg=False):
    nc = bass.Bass("TRN2", target_bir_lowering=False)
    U = min(1024, S)
    NU = S // U
    TPU = U // 256
    NTB = U // 128
    NTG = U // 512 if U >= 512 else 1
    TGW = min(512, U)

    def din(name, shape):
        return nc.dram_tensor(name, list(shape), F32, kind="ExternalInput").ap()

    x_d = din("x", [NSEQ, S, D])
    cl_d = din("cl", [NSEQ, 128, 8])
    wada_d = din("w_ada", [D, 6 * D])
    bada_d = din("b_ada", [1, 6 * D])
    nmix_d = din("norm_mix", [1, D])
    nffn_d = din("norm_ffn", [1, D])
    nfin_d = din("norm_final", [1, D])
    ssmn_d = din("ssm_norm", [1, 2048])
    dtb_d = din("dt_bias", [1, 32])
    alog_d = din("a_log", [1, 32])
    dsk_d = din("d_skip", [1, 32])
    win_d = din("w_in", [D, DIN])
    cw_d = din("cw", [128, 32, 4])
    cb_d = din("cb", [128, 32])
    wro_d = din("w_ret_out", [2048, D])
    wso_d = din("w_ssm_out", [2048, D])
    wo_d = din("w_out", [D, D])
    wr_d = din("w_router", [D, NEXP])
    br_d = din("b_router", [1, NEXP])
    wgu_d = din("w_gate_up", [NEXP, D, 2 * D])
    bgu_d = din("bgu", [128, NEXP, 16])
    wd_d = din("w_down", [NEXP, D, D])
    bd_d = din("b_down", [NEXP, D])
    rope_d = din("rope", [S // 256, 128, 1024])
    decinT_d = din("decinT", [128, 512])
    decq_d = din("decq", [128, 512])
    deck_d = din("deck", [128, 4])
    tri_d = din("tri", [128, 128])
    nm_d = din("nm", [128, 512])
    identf_d = din("identf", [128, 128])
    out_d = nc.dram_tensor("out", [NSEQ, S, D], F32, kind="ExternalOutput").ap()
    dbg_d = {}
    if dbg:
        for nm_, shp in (("d_x1", [NSEQ, S, D]), ("d_retT", [128, 16, 256]), ("d_yT", [128, 16, 256]),
                         ("d_gw", [128, NTB, 32]), ("d_hT", [128, 8, 256]), ("d_qT", [128, 8, 256]),
                         ("d_kT", [128, 8, 256]), ("d_v", [128, 2, 2048]), ("d_mod", [128, 3, 1024]),
                         ("d_xs", [128, 2, 2048]), ("d_dt", [128, 2, 32]), ("d_bmT", [128, 8, 256]),
                         ("d_cmT", [128, 8, 256])):
            dbg_d[nm_] = nc.dram_tensor(nm_, shp, F32, kind="ExternalOutput").ap()

    with ExitStack() as es:
        k = Sched(nc, es)
        PFl = [k.ps("pf%d" % i, [128, 512], F32) for i in range(6)]
        PBl = [k.ps("pb%d" % i, [128, 1024], BF16) for i in range(2)]
        st = {"pf": 0, "pb": 0, "w": 0, "dbgc": 0}

        def pf():
            t = PFl[st["pf"] % 6]
            st["pf"] += 1
            assert t.w is None or len(t.r) > 0, "psum bank reused before consumption"
            return t

        def pb():
            t = PBl[st["pb"] % 2]
            st["pb"] += 1
            assert t.w is None or len(t.r) > 0
            return t

        def bc(ap, n=128):
            return ap.partition_broadcast(n)

        def dbg_out(name, src_ap_fn, tl, shape, is_bf=False):
            if not dbg or name not in dbg_d:
                return
            if "stg" not in st:
                st["stg"] = k.sb("dbgstg", [128, 4096], F32, st["esm"])
            stg = st["stg"]
            n = int(np.prod(shape[1:]))
            if len(shape) == 3:
                sv = stg[:, 0:n].rearrange("p (a b) -> p a b", a=shape[1])
            else:
                sv = stg[:, 0:n]
            k.op("dve", lambda e: e.tensor_copy(out=sv, in_=src_ap_fn()), reads=[tl], writes=[stg])
            k.dma("sp", lambda e: e.dma_start(out=dbg_d[name], in_=sv), reads=[stg])

        ident_b = k.sb("ident_b", [128, 128], BF16)
        ident_f = k.sb("ident_f", [128, 128], F32)
        ones_f = k.sb("ones_f", [128, 128], F32)
        ones_b = k.sb("ones_b", [1, 128], BF16)
        tri_f = k.sb("tri_f", [128, 128], F32)
        nm_f = k.sb("nm_f", [128, 512], F32)
        decinT = k.sb("decinT", [128, 512], F32)
        decq = k.sb("decq", [128, 512], F32)
        deck = k.sb("deck", [128, 4], F32)
        cw = k.sb("cw", [128, 32, 4], F32)
        cb = k.sb("cb", [128, 32], F32)
        a_bc = k.sb("a_bc", [128, 32], F32)
        dtb_bc = k.sb("dtb_bc", [128, 32], F32)
        dsk_bc = k.sb("dsk_bc", [128, 32], F32)
        ssmn_bc = k.sb("ssmn_bc", [128, 2048], BF16)
        epsb = k.sb("epsb", [128, 1], F32)
        oneb = k.sb("oneb", [128, 1], F32)

        k.dma("pool", lambda e: e.dma_start(out=ident_b[:], in_=identf_d), writes=[ident_b])
        k.dma("pool", lambda e: e.dma_start(out=ssmn_bc[:], in_=bc(ssmn_d)), writes=[ssmn_bc])
        for (t_, d_) in ((ident_f, identf_d), (tri_f, tri_d), (nm_f, nm_d), (decinT, decinT_d), (decq, decq_d),
                         (deck, deck_d), (cw, cw_d), (cb, cb_d)):
            k.dma("sp", (lambda t_, d_: (lambda e: e.dma_start(out=t_[:], in_=d_)))(t_, d_), writes=[t_])
        for (t_, d_) in ((a_bc, alog_d), (dtb_bc, dtb_d), (dsk_bc, dsk_d)):
            k.dma("sp", (lambda t_, d_: (lambda e: e.dma_start(out=t_[:], in_=bc(d_))))(t_, d_), writes=[t_])
        k.op("dve", lambda e: e.memset(ones_f[:], 1.0), writes=[ones_f])
        k.op("dve", lambda e: e.memset(ones_b[:], 1.0), writes=[ones_b])
        k.op("dve", lambda e: e.memset(epsb[:], EPS), writes=[epsb])
        k.op("dve", lambda e: e.memset(oneb[:], 1.0), writes=[oneb])
        k.op("act", lambda e: e.activation(out=a_bc[:], in_=a_bc[:], func=AF.Exp), reads=[a_bc], writes=[a_bc])
        k.op("dve", lambda e: e.tensor_scalar(out=a_bc[:], in0=a_bc[:], scalar1=-1.0, scalar2=None, op0=ALU.mult),
             reads=[a_bc], writes=[a_bc])

        wscr_d = nc.dram_tensor("wscr", [38, 128, 4096], BF16, kind="Internal").ap()
        scrT = Tl(None, "wscr")
        blocks = []
        for n in range(24):
            blocks.append(win_d[:, n * 512:(n + 1) * 512])
        for j in range(4):
            blocks.append(win_d[:, 12320 + j * 512:12320 + (j + 1) * 512])
        for wsrc in (wro_d, wso_d):
            for nb in range(2):
                for kh in range(2):
                    blocks.append(wsrc[kh * 1024:(kh + 1) * 1024, nb * 512:(nb + 1) * 512])
        for nb in range(2):
            blocks.append(wo_d[:, nb * 512:(nb + 1) * 512])
        with ExitStack() as esp:
            pstg = [k.sb("pstg%d" % i, [128, 8, 512], F32, esp) for i in range(3)]
            pcb = [k.sb("pcb%d" % i, [128, 8, 512], BF16, esp) for i in range(3)]
            for i, src in enumerate(blocks):
                sg_, cb_ = pstg[i % 3], pcb[i % 3]
                k.dma("sp", lambda e: e.dma_start(out=sg_[:], in_=src.rearrange("(kc p) c -> p kc c", p=128)), writes=[sg_])
                if i % 2 == 0:
                    k.op("act", lambda e: e.copy(out=cb_[:], in_=sg_[:]), reads=[sg_], writes=[cb_])
                else:
                    k.op("dve", lambda e: e.tensor_copy(out=cb_[:], in_=sg_[:]), reads=[sg_], writes=[cb_])
                k.dma("sp", lambda e: e.dma_start(out=wscr_d[i], in_=cb_[:].rearrange("p a b -> p (a b)")), reads=[cb_], writes=[scrT])
            k.barrier()
        if os.environ.get("MK_STOP") == "precast":
            k.finish()
            return nc

        srf_d = nc.dram_tensor("st_rf", [128, 8 * 512], F32, kind="Internal").ap()
        ssf_d = nc.dram_tensor("st_sf", [128, 2048], F32, kind="Internal").ap()
        shl_d = nc.dram_tensor("st_hl", [128, 96], F32, kind="Internal").ap()
        acc = k.sb("acc", [128, NTB, D], F32)
        modm = [k.sb("modm%d" % i, [128, D], F32) for i in range(3)]
        cs = k.sb("cs", [128, 8], F32)
        brow = k.sb("brow", [1, 256], F32)
        ssq = k.sb("ssq", [128, 8], F32)
        rstd = k.sb("rstd", [128, 8], F32)
        sqj = k.sb("sqj", [128, D], BF16)
        hold = {"htmp": None}
        hbf = k.sb("hbf", [128, D], BF16)

        def rms_rstd(src_ap, src_tl, ncols, col):
            k.op("dve", lambda e: e.memset(ssq[:, col:col + 1], 0.0), writes=[ssq])
            k.op("act", lambda e: e.activation(out=sqj[:, 0:ncols], in_=src_ap, func=AF.Square,
                                               accum_out=ssq[:, col:col + 1]),
                 reads=[src_tl, ssq], writes=[sqj, ssq])
            k.op("act", lambda e: e.activation(out=rstd[:, col:col + 1], in_=ssq[:, col:col + 1], func=AF.Ln,
                                               scale=1.0 / ncols, bias=epsb[:]), reads=[ssq, epsb], writes=[rstd])
            k.op("act", lambda e: e.activation(out=rstd[:, col:col + 1], in_=rstd[:, col:col + 1], func=AF.Exp,
                                               scale=-0.5), reads=[rstd], writes=[rstd])

        def norm_mod_T(tb, gamma, shift, dstT, dcol):
            rms_rstd(acc[:, tb, :], acc, D, 0)
            htmp = hold["htmp"]
            k.op("dve", lambda e: e.scalar_tensor_tensor(out=htmp[:], in0=acc[:, tb, :], scalar=rstd[:, 0:1],
                                                         in1=gamma[:], op0=ALU.mult, op1=ALU.mult),
                 reads=[acc, rstd, gamma], writes=[htmp])
            k.op("dve", lambda e: e.tensor_tensor(out=hbf[:], in0=htmp[:], in1=shift[:], op=ALU.add),
                 reads=[htmp, shift], writes=[hbf])
            P = pb()

            def fn(e):
                for j in range(8):
                    ins = e.transpose(P[:, j * 128:(j + 1) * 128], hbf[:, j * 128:(j + 1) * 128], ident_b[:])
                return ins
            k.op("pe", fn, reads=[hbf, ident_b], writes=[P])
            k.op("act", lambda e: e.copy(out=dstT[:, :, dcol:dcol + 128],
                                         in_=P[:].rearrange("p (j t) -> p j t", j=8)), reads=[P], writes=[dstT])

        def compute_mod(b, j0, dst, condbc, wst):
            for i in range(3):
                for q4 in range(4):
                    c0 = (j0 + i) * D + q4 * 256
                    k.dma("sp", lambda e: e.dma_start(out=wst[:], in_=wada_d[:, c0:c0 + 256].rearrange(
                        "(kc p) c -> p kc c", p=128)), writes=[wst])
                    k.dma("sp", lambda e: e.dma_start(out=brow[:], in_=bada_d[0:1, c0:c0 + 256]), writes=[brow])
                    P = pf()

                    def fn(e):
                        for kc in range(8):
                            e.matmul(P[:, 0:256], lhsT=condbc[:, kc, :], rhs=wst[:, kc, :], start=(kc == 0), stop=False)
                        return e.matmul(P[:, 0:256], lhsT=ones_f[0:1, :], rhs=brow[0:1, :], start=False, stop=True)
                    k.op("pe", fn, reads=[condbc, wst, ones_f, brow], writes=[P])
                    k.op("act", lambda e: e.copy(out=dst[i][:, q4 * 256:(q4 + 1) * 256], in_=P[:, 0:256]),
                         reads=[P], writes=[dst[i]])

        def wload(es_unused, wb, blk, ncols=512):
            t = wb[st["w"] % len(wb)]
            st["w"] += 1
            k.dma("sp", lambda e: e.dma_start(out=t[:].rearrange("p a b -> p (a b)"), in_=wscr_d[blk]), reads=[scrT], writes=[t])
            return t

        for b in range(NSEQ):
            k.dma("sp", lambda e: e.dma_start(out=cs[:], in_=cl_d[b]), writes=[cs])
            k.op("act", lambda e: e.activation(out=cs[:], in_=cs[:], func=AF.Silu), reads=[cs], writes=[cs])
            with ExitStack() as esi:
                condbc = k.sb("condbc", [128, 8, 128], F32, esi)
                wst = k.sb("wst", [128, 8, 256], F32, esi)
                nrm_bc = k.sb("nrm_bc", [128, D], F32, esi)
                k.op("dve", lambda e: e.tensor_copy(out=condbc[:], in_=cs[:].unsqueeze(2).broadcast_to([128, 8, 128])),
                     reads=[cs], writes=[condbc])
                compute_mod(b, 0, modm, condbc, wst)
                k.dma("sp", lambda e: e.dma_start(out=nrm_bc[:], in_=bc(nmix_d)), writes=[nrm_bc])
                k.op("dve", lambda e: e.scalar_tensor_tensor(out=modm[1][:], in0=modm[1][:], scalar=1.0, in1=nrm_bc[:],
                                                             op0=ALU.add, op1=ALU.mult), reads=[modm[1], nrm_bc], writes=[modm[1]])
                k.barrier()
            if dbg and b == 0:
                for i in range(3):
                    k.dma("sp", (lambda i: (lambda e: e.dma_start(out=dbg_d["d_mod"][:, i, :], in_=modm[i][:])))(i),
                          reads=[modm[i]])

            for u in range(NU):
                with ExitStack() as esm:
                    st["esm"] = esm
                    st.pop("stg", None)
                    hT = k.sb("hT", [128, 8, 256], BF16, esm)
                    wb = [k.sb("wb%d" % i, [128, 8, 512], BF16, esm) for i in range(3)]
                    R1 = k.sb("R1", [128, 12288], BF16, esm)
                    R2 = k.sb("R2", [128, 4096], BF16, esm)

                    def vw(T, a, b_, n):
                        return V(T, T[:, a:b_].rearrange("p (a b) -> p a b", a=n))
                    qT = vw(R1, 0, 2048, 8)
                    qsT = vw(R1, 2048, 4096, 8)
                    kT = vw(R1, 4096, 6144, 8)
                    ktm = vw(R1, 6144, 8192, 2)
                    vtm = vw(R1, 8192, 12288, 2)
                    xs = vw(R1, 0, 4096, 2)
                    bmT = vw(R1, 4096, 6144, 8)
                    bmtm = vw(R1, 6144, 8192, 2)
                    cmT = vw(R1, 8192, 10240, 8)
                    gaT = vw(R1, 0, 2048, 8)
                    gbT = vw(R1, 2048, 4096, 8)
                    mgT = vw(R1, 4096, 6144, 8)
                    retT = vw(R2, 0, 4096, 16)
                    yT = vw(R2, 0, 4096, 16)
                    sg = k.sb("sg", [128, 2, 2048], BF16, esm)
                    maT = k.sb("maT", [128, 8, 256], BF16, esm)
                    rst_f = k.sb("rst_f", [128, 8, 512], F32, esm)
                    rst_b = k.sb("rst_b", [128, 8, 512], BF16, esm)
                    sst_f = [k.sb("sst_f%d" % i, [128, 256], F32, esm) for i in range(8)]
                    sst_b = [k.sb("sst_b%d" % i, [128, 256], BF16, esm) for i in range(8)]
                    halo = k.sb("halo", [128, 32, 3], F32, esm)
                    if u == 0:
                        for t_ in [rst_f, rst_b, halo] + sst_f + sst_b:
                            k.op("dve", (lambda t_: (lambda e: e.memset(t_[:], 0.0)))(t_), writes=[t_])
                    else:
                        k.dma("sp", lambda e: e.dma_start(out=rst_f[:].rearrange("p a b -> p (a b)"), in_=srf_d), writes=[rst_f])
                        for i in range(8):
                            k.dma("sp", lambda e: e.dma_start(out=sst_f[i][:], in_=ssf_d[:, i * 256:(i + 1) * 256]), writes=[sst_f[i]])
                        k.dma("sp", lambda e: e.dma_start(out=halo[:].rearrange("p a b -> p (a b)"), in_=shl_d), writes=[halo])
                        k.op("act", lambda e: e.copy(out=rst_b[:], in_=rst_f[:]), reads=[rst_f], writes=[rst_b])
                        for i in range(8):
                            k.op("act", lambda e: e.copy(out=sst_b[i][:], in_=sst_f[i][:]), reads=[sst_f[i]], writes=[sst_b[i]])
                    ropet = k.sb("ropet", [128, 1024], F32, esm)
                    print("sbuf remaining (mixer, before temps)", nc.sbuf_bytes_remaining, flush=True)
                    rAB = k.sb("rAB", [128, 1024], F32, esm)
                    rA = V(rAB, rAB[:, 0:512])
                    rB = V(rAB, rAB[:, 512:1024])
                    hold["htmp"] = V(rAB, rAB[:, 0:1024])
                    sm = k.sb("sm", [128, 512], BF16, esm)
                    rg = k.sb("rg", [128, 512], BF16, esm)
                    craw = [k.sb("craw%d" % i, [128, 259], F32, esm) for i in range(2)]
                    cacc = [k.sb("cacc%d" % i, [128, 256], F32, esm) for i in range(2)]
                    cso = [k.sb("cso%d" % i, [128, 256], BF16, esm) for i in range(2)]
                    dtt = k.sb("dtt", [128, 2, 32], F32, esm)
                    adt = k.sb("adt", [128, 32], F32, esm)
                    acs = k.sb("acs", [128, 32], F32, esm)
                    nacs = k.sb("nacs", [128, 32], F32, esm)
                    eacs = k.sb("eacs", [128, 32], F32, esm)
                    etot = k.sb("etot", [128, 32], F32, esm)
                    dS = k.sb("dS", [128, 32], F32, esm)
                    cbT = k.sb("cbT", [128, 8, 128], BF16, esm)
                    Xg = [k.sb("Xg%d" % i, [128, 4, 128], F32, esm) for i in range(2)]
                    seg = [k.sb("seg%d" % i, [128, 4, 128], BF16, esm) for i in range(2)]
                    ssq2 = [k.sb("ssq2%d" % i, [128, 1], F32, esm) for i in range(2)]
                    rstd2 = [k.sb("rstd2%d" % i, [128, 1], F32, esm) for i in range(2)]
                    sqj2 = [k.sb("sqj2%d" % i, [128, 256], BF16, esm) for i in range(2)]
                    Mg = [k.sb("Mg%d" % i, [128, 4, 128], BF16, esm) for i in range(2)]
                    xdt = [k.sb("xdt%d" % i, [128, 256], BF16, esm) for i in range(2)]
                    xdS = [k.sb("xdS%d" % i, [128, 256], BF16, esm) for i in range(2)]
                    yt1_ = [k.sb("yt1%d" % i, [128, 256], F32, esm) for i in range(2)]
                    yt2_ = [k.sb("yt2%d" % i, [128, 256], F32, esm) for i in range(2)]
                    ynb_ = [k.sb("ynb%d" % i, [128, 256], BF16, esm) for i in range(2)]
                    wdt = k.sb("wdt", [128, 8, 32], BF16, esm)
                    otmp = rA
                    print("sbuf remaining (mixer)", nc.sbuf_bytes_remaining, flush=True)

                    k.dma("pool", lambda e: e.dma_start(out=wdt[:], in_=win_d[:, 12288:12320].rearrange(
                        "(kc p) c -> p kc c", p=128)), writes=[wdt])

                    for tt in range(TPU):
                        gt = u * TPU + tt
                        tok0 = gt * 256
                        tb0 = tt * 2
                        k.dma("sp", lambda e: e.dma_start(out=acc[:, tb0:tb0 + 2, :], in_=x_d[b, tok0:tok0 + 256, :].rearrange(
                            "(tc p) d -> p tc d", p=128)), writes=[acc])
                        k.dma("sp", lambda e: e.dma_start(out=ropet[:], in_=rope_d[gt]), writes=[ropet])
                        for tc in range(2):
                            norm_mod_T(tb0 + tc, modm[1], modm[0], hT, tc * 128)
                        if dbg and b == 0 and gt == 0:
                            dbg_out("d_hT", lambda: hT[:], hT, [128, 8, 256])

                        def fm_group(W, cc, P, col0):
                            def fn(e):
                                for kc in range(8):
                                    ins = e.matmul(P[:, col0:col0 + 256], lhsT=W[:, kc, cc * 128:(cc + 1) * 128],
                                                   rhs=hT[:, kc, :], start=(kc == 0), stop=(kc == 7))
                                return ins
                            k.op("pe", fn, reads=[W, hT], writes=[P])

                        def tm_group(W, tc, P, ncols):
                            def fn(e):
                                for kc in range(8):
                                    ins = e.matmul(P[:, 0:ncols], lhsT=hT[:, kc, tc * 128:(tc + 1) * 128],
                                                   rhs=W[:, kc, 0:ncols], start=(kc == 0), stop=(kc == 7))
                                return ins
                            k.op("pe", fn, reads=[W, hT], writes=[P])

                        for (blk0, dstT) in ((0, qT), (2, kT)):
                            for bi in range(2):
                                W = wload(esm, wb, blk0 + bi)
                                for hp in range(2):
                                    h = bi * 2 + hp
                                    P = pf()
                                    fm_group(W, hp * 2, P, 0)
                                    fm_group(W, hp * 2 + 1, P, 256)
                                    k.op("dve", lambda e: e.tensor_tensor(out=rA[:], in0=P[:], in1=ropet[:, 0:512], op=ALU.mult),
                                         reads=[P, ropet], writes=[rA])
                                    k.op("dve", lambda e: e.tensor_tensor(out=rB[:], in0=P[:], in1=ropet[:, 512:1024], op=ALU.mult),
                                         reads=[P, ropet], writes=[rB])
                                    k.op("dve", lambda e: e.tensor_tensor(out=dstT[:, 2 * h, :], in0=rA[:, 0:256], in1=rA[:, 256:512],
                                                                          op=ALU.subtract), reads=[rA], writes=[dstT])
                                    k.op("dve", lambda e: e.tensor_tensor(out=dstT[:, 2 * h + 1, :], in0=rB[:, 0:256], in1=rB[:, 256:512],
                                                                          op=ALU.add), reads=[rB], writes=[dstT])
                        for h in range(4):
                            for dc in range(2):
                                k.op("dve", lambda e: e.tensor_tensor(
                                    out=qsT[:, 2 * h + dc, :].rearrange("p (a l) -> p a l", a=2),
                                    in0=qT[:, 2 * h + dc, :].rearrange("p (a l) -> p a l", a=2),
                                    in1=decq[:, h * 128:(h + 1) * 128].unsqueeze(1).broadcast_to([128, 2, 128]),
                                    op=ALU.mult), reads=[qT, decq], writes=[qsT])
                        for tc in range(2):
                            P = pb()

                            def fn(e):
                                for j in range(8):
                                    ins = e.transpose(P[:, j * 128:(j + 1) * 128], kT[:, j, tc * 128:(tc + 1) * 128], ident_b[:])
                                return ins
                            k.op("pe", fn, reads=[kT, ident_b], writes=[P])
                            for h in range(4):
                                k.op("act", lambda e: e.activation(out=ktm[:, tc, h * 256:(h + 1) * 256], in_=P[:, h * 256:(h + 1) * 256],
                                                                   func=AF.Copy, scale=deck[:, h:h + 1]), reads=[P, deck], writes=[ktm])
                        if dbg and b == 0 and gt == 0:
                            dbg_out("d_qT", lambda: qT[:], qT, [128, 8, 256])
                            dbg_out("d_kT", lambda: kT[:], kT, [128, 8, 256])
                        for bi in range(4):
                            W = wload(esm, wb, 4 + bi)
                            for tc in range(2):
                                P = pf()
                                tm_group(W, tc, P, 512)
                                k.op("act", lambda e: e.copy(out=vtm[:, tc, bi * 512:(bi + 1) * 512], in_=P[:]), reads=[P], writes=[vtm])
                        for bi in range(4):
                            W = wload(esm, wb, 8 + bi)
                            for tc in range(2):
                                P = pf()
                                tm_group(W, tc, P, 512)
                                k.op("act", lambda e: e.activation(out=sg[:, tc, bi * 512:(bi + 1) * 512], in_=P[:], func=AF.Silu),
                                     reads=[P], writes=[sg])
                        if dbg and b == 0 and gt == 0:
                            dbg_out("d_v", lambda: vtm[:], vtm, [128, 2, 2048])
                        for tc in range(2):
                            tsl = slice(tc * 128, (tc + 1) * 128)
                            Ps = pf()

                            def fn(e):
                                for h in range(4):
                                    for dc in range(2):
                                        ins = e.matmul(Ps[:, h * 128:(h + 1) * 128], lhsT=kT[:, 2 * h + dc, tsl], rhs=qT[:, 2 * h + dc, tsl],
                                                       start=(dc == 0), stop=(dc == 1))
                                return ins
                            k.op("pe", fn, reads=[kT, qT], writes=[Ps])
                            k.op("dve", lambda e: e.tensor_tensor(out=sm[:], in0=Ps[:], in1=decinT[:], op=ALU.mult),
                                 reads=[Ps, decinT], writes=[sm])
                            for h in range(4):
                                Pr = pf()

                                def fn(e):
                                    e.matmul(Pr[:], lhsT=sm[:, h * 128:(h + 1) * 128], rhs=vtm[:, tc, h * 512:(h + 1) * 512],
                                             start=True, stop=False)
                                    e.matmul(Pr[:], lhsT=qsT[:, 2 * h, tsl], rhs=rst_b[:, 2 * h, :], start=False, stop=False)
                                    return e.matmul(Pr[:], lhsT=qsT[:, 2 * h + 1, tsl], rhs=rst_b[:, 2 * h + 1, :], start=False, stop=True)
                                k.op("pe", fn, reads=[sm, vtm, qsT, rst_b], writes=[Pr])
                                rms_rstd(Pr[:], Pr, 512, 1)
                                k.op("dve", lambda e: e.scalar_tensor_tensor(out=rg[:], in0=Pr[:], scalar=rstd[:, 1:2],
                                                                             in1=sg[:, tc, h * 512:(h + 1) * 512], op0=ALU.mult, op1=ALU.mult),
                                     reads=[Pr, rstd, sg], writes=[rg])
                                Pt = pb()

                                def fn(e):
                                    for j in range(4):
                                        ins = e.transpose(Pt[:, j * 128:(j + 1) * 128], rg[:, j * 128:(j + 1) * 128], ident_b[:])
                                    return ins
                                k.op("pe", fn, reads=[rg, ident_b], writes=[Pt])
                                k.op("act", lambda e: e.copy(out=retT[:, 4 * h:4 * h + 4, tsl],
                                                             in_=Pt[:, 0:512].rearrange("p (j t) -> p j t", j=4)), reads=[Pt], writes=[retT])
                                for dc in range(2):
                                    Pu = pf()
                                    k.op("pe", lambda e: e.matmul(Pu[:], lhsT=ktm[:, tc, h * 256 + dc * 128:h * 256 + (dc + 1) * 128],
                                                                  rhs=vtm[:, tc, h * 512:(h + 1) * 512], start=True, stop=True),
                                         reads=[ktm, vtm], writes=[Pu])
                                    k.op("dve", lambda e: e.scalar_tensor_tensor(out=rst_f[:, 2 * h + dc, :], in0=rst_f[:, 2 * h + dc, :],
                                                                                 scalar=gC[h], in1=Pu[:], op0=ALU.mult, op1=ALU.add),
                                         reads=[rst_f, Pu], writes=[rst_f])
                                    k.op("act", lambda e: e.copy(out=rst_b[:, 2 * h + dc, :], in_=rst_f[:, 2 * h + dc, :]),
                                         reads=[rst_f], writes=[rst_b])
                        if dbg and b == 0 and gt == 0:
                            dbg_out("d_retT", lambda: retT[:], retT, [128, 16, 256])
                        def out_projT(blk_base, srcT, dst_f32, first):
                            for nb in range(2):
                                Pa = [pf(), pf()]
                                Ws = [wload(esm, wb, blk_base + nb * 2 + kh) for kh in range(2)]

                                def fn(e):
                                    for cc in range(4):
                                        for kh in range(2):
                                            for kc in range(8):
                                                ins = e.matmul(Pa[cc // 2][:, (cc % 2) * 256:(cc % 2 + 1) * 256],
                                                               lhsT=Ws[kh][:, kc, cc * 128:(cc + 1) * 128], rhs=srcT[:, kh * 8 + kc, :],
                                                               start=(kh == 0 and kc == 0), stop=(kh == 1 and kc == 7))
                                    return ins
                                k.op("pe", fn, reads=[Ws[0], Ws[1], srcT], writes=[Pa[0], Pa[1]])
                                for half in range(2):
                                    cc0 = nb * 4 + half * 2
                                    k.op("act", lambda e: e.copy(out=dst_f32[:, cc0:cc0 + 2, :],
                                                                 in_=Pa[half][:].rearrange("p (c t) -> p c t", c=2)),
                                         reads=[Pa[half]], writes=[dst_f32])
                        out_projT(28, retT, maT, True)

                        for bi in range(4):
                            W = wload(esm, wb, 12 + bi)
                            for tc in range(2):
                                P = pf()
                                tm_group(W, tc, P, 512)
                                k.op("act", lambda e: e.activation(out=sg[:, tc, bi * 512:(bi + 1) * 512], in_=P[:], func=AF.Silu),
                                     reads=[P], writes=[sg])
                        pend = [None]

                        def mk_s2(cc, ca, co):
                            def s2():
                                if cc < 16:
                                    k.op("act", lambda e: e.activation(out=co[:], in_=ca[:], func=AF.Silu), reads=[ca], writes=[co])
                                    Pt = pb()

                                    def fn(e):
                                        for tc in range(2):
                                            ins = e.transpose(Pt[:, tc * 128:(tc + 1) * 128], co[:, tc * 128:(tc + 1) * 128], ident_b[:])
                                        return ins
                                    k.op("pe", fn, reads=[co, ident_b], writes=[Pt])
                                    k.op("act", lambda e: e.copy(out=xs[:, :, cc * 128:(cc + 1) * 128],
                                                                 in_=Pt[:, 0:256].rearrange("p (a t) -> p a t", a=2)),
                                         reads=[Pt], writes=[xs])
                                elif cc < 24:
                                    g_ = cc - 16
                                    k.op("act", lambda e: e.activation(out=bmT[:, g_, :], in_=ca[:], func=AF.Silu), reads=[ca], writes=[bmT])
                                    Pt = pb()

                                    def fn(e):
                                        for tc in range(2):
                                            ins = e.transpose(Pt[:, tc * 128:(tc + 1) * 128], bmT[:, g_, tc * 128:(tc + 1) * 128], ident_b[:])
                                        return ins
                                    k.op("pe", fn, reads=[bmT, ident_b], writes=[Pt])
                                    k.op("act", lambda e: e.copy(out=bmtm[:, :, g_ * 128:(g_ + 1) * 128],
                                                                 in_=Pt[:, 0:256].rearrange("p (a t) -> p a t", a=2)),
                                         reads=[Pt], writes=[bmtm])
                                else:
                                    g_ = cc - 24
                                    k.op("act", lambda e: e.activation(out=cmT[:, g_, :], in_=ca[:], func=AF.Silu), reads=[ca], writes=[cmT])
                            return s2

                        for bi in range(8):
                            W = wload(esm, wb, 16 + bi)
                            for half in range(2):
                                P = pf()
                                fm_group(W, half * 2, P, 0)
                                fm_group(W, half * 2 + 1, P, 256)
                                for c2 in range(2):
                                    cc = bi * 4 + half * 2 + c2
                                    cr = craw[cc % 2]
                                    ca = cacc[cc % 2]
                                    co = cso[cc % 2]
                                    k.op("act", lambda e: e.copy(out=cr[:, 0:3], in_=halo[:, cc, :]), reads=[halo], writes=[cr])
                                    k.op("act", lambda e: e.copy(out=cr[:, 3:259], in_=P[:, c2 * 256:(c2 + 1) * 256]), reads=[P], writes=[cr])
                                    k.op("act", lambda e: e.copy(out=halo[:, cc, :], in_=cr[:, 256:259]), reads=[cr], writes=[halo])
                                    k.op("dve", lambda e: e.tensor_scalar(out=ca[:], in0=cr[:, 3:259], scalar1=cw[:, cc, 3:4],
                                                                          scalar2=cb[:, cc:cc + 1], op0=ALU.mult, op1=ALU.add),
                                         reads=[cr, cw, cb], writes=[ca])
                                    for kk in range(3):
                                        k.op("dve", (lambda kk: (lambda e: e.scalar_tensor_tensor(
                                            out=ca[:], in0=cr[:, kk:kk + 256], scalar=cw[:, cc, kk:kk + 1], in1=ca[:],
                                            op0=ALU.mult, op1=ALU.add)))(kk), reads=[cr, cw, ca], writes=[ca])
                                    if pend[0] is not None:
                                        pend[0]()
                                    pend[0] = mk_s2(cc, ca, co)
                        pend[0]()
                        pend[0] = None
                        for tc in range(2):
                            P = pf()
                            tm_group(wdt, tc, P, 32)
                            k.op("dve", lambda e: e.tensor_tensor(out=dtt[:, tc, :], in0=P[:, 0:32], in1=dtb_bc[:], op=ALU.add),
                                 reads=[P, dtb_bc], writes=[dtt])
                        k.op("act", lambda e: e.activation(out=dtt[:], in_=dtt[:], func=AF.Exp), reads=[dtt], writes=[dtt])
                        k.op("act", lambda e: e.activation(out=dtt[:], in_=dtt[:], func=AF.Ln, bias=oneb[:]), reads=[dtt, oneb], writes=[dtt])
                        if dbg and b == 0 and gt == 0:
                            dbg_out("d_xs", lambda: xs[:], xs, [128, 2, 2048])
                            dbg_out("d_dt", lambda: dtt[:], dtt, [128, 2, 32])
                            dbg_out("d_bmT", lambda: bmT[:], bmT, [128, 8, 256])
                            dbg_out("d_cmT", lambda: cmT[:], cmT, [128, 8, 256])
                        for tc in range(2):
                            tsl = slice(tc * 128, (tc + 1) * 128)
                            k.op("dve", lambda e: e.tensor_tensor(out=adt[:], in0=dtt[:, tc, :], in1=a_bc[:], op=ALU.mult),
                                 reads=[dtt, a_bc], writes=[adt])
                            P = pf()
                            k.op("pe", lambda e: e.matmul(P[:, 0:32], lhsT=tri_f[:], rhs=adt[:], start=True, stop=True),
                                 reads=[tri_f, adt], writes=[P])
                            P2 = pf()
                            k.op("pe", lambda e: e.matmul(P2[:, 0:32], lhsT=ones_f[:], rhs=adt[:], start=True, stop=True),
                                 reads=[ones_f, adt], writes=[P2])
                            k.op("act", lambda e: e.copy(out=acs[:], in_=P[:, 0:32]), reads=[P], writes=[acs])
                            k.op("dve", lambda e: e.tensor_scalar(out=nacs[:], in0=P[:, 0:32], scalar1=-1.0, scalar2=None, op0=ALU.mult),
                                 reads=[P], writes=[nacs])
                            k.op("act", lambda e: e.activation(out=eacs[:], in_=P[:, 0:32], func=AF.Exp), reads=[P], writes=[eacs])
                            k.op("act", lambda e: e.activation(out=etot[:], in_=P2[:, 0:32], func=AF.Exp), reads=[P2], writes=[etot])
                            k.op("dve", lambda e: e.tensor_tensor(out=dS[:], in0=P2[:, 0:32], in1=acs[:], op=ALU.subtract),
                                 reads=[P2, acs], writes=[dS])
                            k.op("act", lambda e: e.activation(out=dS[:], in_=dS[:], func=AF.Exp), reads=[dS], writes=[dS])
                            for hf in range(2):
                                Pc = pf()

                                def fn(e):
                                    for gg in range(4):
                                        g_ = hf * 4 + gg
                                        ins = e.matmul(Pc[:, gg * 128:(gg + 1) * 128], lhsT=bmT[:, g_, tsl], rhs=cmT[:, g_, tsl], start=True, stop=True)
                                    return ins
                                k.op("pe", fn, reads=[bmT, cmT], writes=[Pc])
                                k.op("act", lambda e: e.copy(out=cbT[:, hf * 4:(hf + 1) * 4, :], in_=Pc[:].rearrange("p (g l) -> p g l", g=4)),
                                     reads=[Pc], writes=[cbT])
                            def stA(g_):
                                X, sgm, M, xd, xS = Xg[g_ % 2], seg[g_ % 2], Mg[g_ % 2], xdt[g_ % 2], xdS[g_ % 2]
                                hs = slice(4 * g_, 4 * g_ + 4)
                                cs_ = slice(g_ * 256, (g_ + 1) * 256)
                                k.op("dve", lambda e: e.tensor_tensor(out=X[:], in0=tri_f[:].unsqueeze(1).broadcast_to([128, 4, 128]),
                                                                      in1=adt[:, hs].unsqueeze(2).broadcast_to([128, 4, 128]), op=ALU.mult),
                                     reads=[tri_f, adt], writes=[X])
                                Pa = pf()

                                def fn(e):
                                    e.matmul(Pa[:], lhsT=ones_f[:], rhs=X[:].rearrange("p a l -> p (a l)"), start=True, stop=False)
                                    return e.matmul(Pa[:], lhsT=ident_f[:], rhs=nm_f[:], start=False, stop=True)
                                k.op("pe", fn, reads=[ones_f, X, ident_f, nm_f], writes=[Pa])
                                for hh in range(4):
                                    k.op("act", lambda e: e.activation(out=sgm[:, hh, :], in_=Pa[:, hh * 128:(hh + 1) * 128], func=AF.Exp,
                                                                       bias=nacs[:, 4 * g_ + hh:4 * g_ + hh + 1]),
                                         reads=[Pa, nacs], writes=[sgm])

                            def stA2(g_):
                                X, sgm, M, xd, xS = Xg[g_ % 2], seg[g_ % 2], Mg[g_ % 2], xdt[g_ % 2], xdS[g_ % 2]
                                hs = slice(4 * g_, 4 * g_ + 4)
                                cs_ = slice(g_ * 256, (g_ + 1) * 256)
                                k.op("dve", lambda e: e.tensor_tensor(out=M[:], in0=sgm[:], in1=cbT[:, g_, :].unsqueeze(1).broadcast_to([128, 4, 128]),
                                                                      op=ALU.mult), reads=[sgm, cbT], writes=[M])
                                k.op("dve", lambda e: e.tensor_tensor(out=xd[:].rearrange("p (h q) -> p h q", h=4),
                                                                      in0=xs[:, tc, cs_].rearrange("p (h q) -> p h q", h=4),
                                                                      in1=dtt[:, tc, hs].unsqueeze(2).broadcast_to([128, 4, 64]), op=ALU.mult),
                                     reads=[xs, dtt], writes=[xd])
                                k.op("dve", lambda e: e.tensor_tensor(out=xS[:].rearrange("p (h q) -> p h q", h=4),
                                                                      in0=xd[:].rearrange("p (h q) -> p h q", h=4),
                                                                      in1=dS[:, hs].unsqueeze(2).broadcast_to([128, 4, 64]), op=ALU.mult),
                                     reads=[xd, dS], writes=[xS])

                            def stB(g_):
                                yt1, yt2, ynb = yt1_[g_ % 2], yt2_[g_ % 2], ynb_[g_ % 2]
                                M, xd = Mg[g_ % 2], xdt[g_ % 2]
                                sq_, rs_, sj_ = ssq2[g_ % 2], rstd2[g_ % 2], sqj2[g_ % 2]
                                hs = slice(4 * g_, 4 * g_ + 4)
                                cs_ = slice(g_ * 256, (g_ + 1) * 256)
                                Py = pf()

                                def fn(e):
                                    for hh in range(4):
                                        e.matmul(Py[:, hh * 64:(hh + 1) * 64], lhsT=M[:, hh, :], rhs=xd[:, hh * 64:(hh + 1) * 64], start=True, stop=True)
                                    return e.matmul(Py[:, 256:512], lhsT=cmT[:, g_, tsl], rhs=sst_b[g_][:], start=True, stop=True)
                                k.op("pe", fn, reads=[M, xd, cmT, sst_b[g_]], writes=[Py])
                                k.op("dve", lambda e: e.tensor_tensor(out=yt1[:].rearrange("p (h q) -> p h q", h=4),
                                                                      in0=Py[:, 256:512].rearrange("p (h q) -> p h q", h=4),
                                                                      in1=eacs[:, hs].unsqueeze(2).broadcast_to([128, 4, 64]), op=ALU.mult),
                                     reads=[Py, eacs], writes=[yt1])
                                k.op("dve", lambda e: e.tensor_tensor(out=yt1[:], in0=yt1[:], in1=Py[:, 0:256], op=ALU.add),
                                     reads=[yt1, Py], writes=[yt1])
                                k.op("dve", lambda e: e.tensor_tensor(out=yt2[:].rearrange("p (h q) -> p h q", h=4),
                                                                      in0=xs[:, tc, cs_].rearrange("p (h q) -> p h q", h=4),
                                                                      in1=dsk_bc[:, hs].unsqueeze(2).broadcast_to([128, 4, 64]), op=ALU.mult),
                                     reads=[xs, dsk_bc], writes=[yt2])
                                k.op("dve", lambda e: e.tensor_tensor(out=yt1[:], in0=yt1[:], in1=yt2[:], op=ALU.add),
                                     reads=[yt1, yt2], writes=[yt1])
                                k.op("dve", lambda e: e.tensor_tensor(out=yt1[:], in0=yt1[:], in1=sg[:, tc, cs_], op=ALU.mult),
                                     reads=[yt1, sg], writes=[yt1])
                                k.op("dve", lambda e: e.memset(sq_[:], 0.0), writes=[sq_])

                            def stB2(g_):
                                yt1, yt2, ynb = yt1_[g_ % 2], yt2_[g_ % 2], ynb_[g_ % 2]
                                sq_, rs_, sj_ = ssq2[g_ % 2], rstd2[g_ % 2], sqj2[g_ % 2]
                                cs_ = slice(g_ * 256, (g_ + 1) * 256)
                                k.op("act", lambda e: e.activation(out=sj_[:], in_=yt1[:], func=AF.Square, accum_out=sq_[:]),
                                     reads=[yt1, sq_], writes=[sj_, sq_])
                                k.op("act", lambda e: e.activation(out=rs_[:], in_=sq_[:], func=AF.Ln, scale=1.0 / 256, bias=epsb[:]),
                                     reads=[sq_, epsb], writes=[rs_])
                                k.op("act", lambda e: e.activation(out=rs_[:], in_=rs_[:], func=AF.Exp, scale=-0.5), reads=[rs_], writes=[rs_])
                                k.op("dve", lambda e: e.scalar_tensor_tensor(out=ynb[:], in0=yt1[:], scalar=rs_[:, 0:1], in1=ssmn_bc[:, cs_],
                                                                             op0=ALU.mult, op1=ALU.mult), reads=[yt1, rs_, ssmn_bc], writes=[ynb])
                                Pt = pb()

                                def fn(e):
                                    for j in range(2):
                                        ins = e.transpose(Pt[:, j * 128:(j + 1) * 128], ynb[:, j * 128:(j + 1) * 128], ident_b[:])
                                    return ins
                                k.op("pe", fn, reads=[ynb, ident_b], writes=[Pt])
                                k.op("act", lambda e: e.copy(out=yT[:, 2 * g_:2 * g_ + 2, tsl], in_=Pt[:, 0:256].rearrange("p (j t) -> p j t", j=2)),
                                     reads=[Pt], writes=[yT])

                            def stC(g_):
                                xS = xdS[g_ % 2]
                                hs = slice(4 * g_, 4 * g_ + 4)
                                Pu = pf()
                                k.op("pe", lambda e: e.matmul(Pu[:, 0:256], lhsT=bmtm[:, tc, g_ * 128:(g_ + 1) * 128], rhs=xS[:], start=True, stop=True),
                                     reads=[bmtm, xS], writes=[Pu])
                                k.op("dve", lambda e: e.tensor_tensor(out=sst_f[g_][:].rearrange("p (h q) -> p h q", h=4),
                                                                      in0=sst_f[g_][:].rearrange("p (h q) -> p h q", h=4),
                                                                      in1=etot[:, hs].unsqueeze(2).broadcast_to([128, 4, 64]), op=ALU.mult),
                                     reads=[sst_f[g_], etot], writes=[sst_f[g_]])
                                k.op("dve", lambda e: e.tensor_tensor(out=sst_f[g_][:], in0=sst_f[g_][:], in1=Pu[:, 0:256], op=ALU.add),
                                     reads=[sst_f[g_], Pu], writes=[sst_f[g_]])
                                k.op("act", lambda e: e.copy(out=sst_b[g_][:], in_=sst_f[g_][:]), reads=[sst_f[g_]], writes=[sst_b[g_]])

                            stA(0)
                            stA2(0)
                            for g_ in range(8):
                                if g_ < 7:
                                    stA(g_ + 1)
                                stB(g_)
                                if g_ < 7:
                                    stA2(g_ + 1)
                                stC(g_)
                                stB2(g_)
                        if dbg and b == 0 and gt == 0:
                            dbg_out("d_yT", lambda: yT[:], yT, [128, 16, 256])
                        for (c0, dstT) in ((24, gaT), (26, gbT)):
                            for bi in range(2):
                                W = wload(esm, wb, c0 + bi)
                                for half in range(2):
                                    P = pf()
                                    fm_group(W, half * 2, P, 0)
                                    fm_group(W, half * 2 + 1, P, 256)
                                    cc0 = bi * 4 + half * 2
                                    k.op("act", lambda e: e.activation(out=dstT[:, cc0:cc0 + 2, :], in_=P[:].rearrange("p (c t) -> p c t", c=2),
                                                                       func=AF.Sigmoid), reads=[P], writes=[dstT])
                        k.op("dve", lambda e: e.tensor_tensor(out=maT[:], in0=maT[:], in1=gaT[:], op=ALU.mult), reads=[maT, gaT], writes=[maT])
                        for nb in range(2):
                            Pa = [pf(), pf()]
                            Ws = [wload(esm, wb, 32 + nb * 2 + kh) for kh in range(2)]

                            def fn(e):
                                for cc in range(4):
                                    for kh in range(2):
                                        for kc in range(8):
                                            ins = e.matmul(Pa[cc // 2][:, (cc % 2) * 256:(cc % 2 + 1) * 256],
                                                           lhsT=Ws[kh][:, kc, cc * 128:(cc + 1) * 128], rhs=yT[:, kh * 8 + kc, :],
                                                           start=(kh == 0 and kc == 0), stop=(kh == 1 and kc == 7))
                                return ins
                            k.op("pe", fn, reads=[Ws[0], Ws[1], yT], writes=[Pa[0], Pa[1]])
                            for half in range(2):
                                cc0 = nb * 4 + half * 2
                                k.op("dve", lambda e: e.tensor_tensor(out=rA[:].rearrange("p (c t) -> p c t", c=2),
                                                                      in0=Pa[half][:].rearrange("p (c t) -> p c t", c=2),
                                                                      in1=gbT[:, cc0:cc0 + 2, :], op=ALU.mult), reads=[Pa[half], gbT], writes=[rA])
                                k.op("dve", lambda e: e.tensor_tensor(out=mgT[:, cc0:cc0 + 2, :], in0=rA[:].rearrange("p (c t) -> p c t", c=2),
                                                                      in1=maT[:, cc0:cc0 + 2, :], op=ALU.add), reads=[rA, maT], writes=[mgT])
                        for nb in range(2):
                            W = wload(esm, wb, 36 + nb)
                            for tc in range(2):
                                P = pf()

                                def fn(e):
                                    for kc in range(8):
                                        ins = e.matmul(P[:], lhsT=mgT[:, kc, tc * 128:(tc + 1) * 128], rhs=W[:, kc, :], start=(kc == 0), stop=(kc == 7))
                                    return ins
                                k.op("pe", fn, reads=[mgT, W], writes=[P])
                                k.op("dve", lambda e: e.tensor_tensor(out=otmp[:], in0=P[:], in1=modm[2][:, nb * 512:(nb + 1) * 512], op=ALU.mult),
                                     reads=[P, modm[2]], writes=[otmp])
                                k.op("dve", lambda e: e.tensor_tensor(out=acc[:, tb0 + tc, nb * 512:(nb + 1) * 512],
                                                                      in0=acc[:, tb0 + tc, nb * 512:(nb + 1) * 512], in1=otmp[:], op=ALU.add),
                                     reads=[acc, otmp], writes=[acc])
                        if dbg:
                            k.dma("sp", lambda e: e.dma_start(out=dbg_d["d_x1"][b, tok0:tok0 + 256, :].rearrange("(tc p) d -> p tc d", p=128),
                                                              in_=acc[:, tb0:tb0 + 2, :]), reads=[acc])
                    if u < NU - 1:
                        k.dma("sp", lambda e: e.dma_start(out=srf_d, in_=rst_f[:].rearrange("p a b -> p (a b)")), reads=[rst_f])
                        for i in range(8):
                            k.dma("sp", lambda e: e.dma_start(out=ssf_d[:, i * 256:(i + 1) * 256], in_=sst_f[i][:]), reads=[sst_f[i]])
                        k.dma("sp", lambda e: e.dma_start(out=shl_d, in_=halo[:].rearrange("p a b -> p (a b)")), reads=[halo])
                    k.barrier()
                if os.environ.get("MK_STOP") == "mixer":
                    k.finish()
                    return nc
                with ExitStack() as ese:
                    hold["htmp"] = k.sb("htmp", [128, D], F32, ese)
                    h2T = k.sb("h2T", [128, 8, U], BF16, ese)
                    actT = k.sb("actT", [128, 8, U], BF16, ese)
                    wgu = [k.sb("wgu%d" % i, [128, 8, 2048], BF16, ese) for i in range(1)]
                    wdn = [k.sb("wdn%d" % i, [128, 8, 1024], BF16, ese) for i in range(1)]
                    bdn = [k.sb("bdn%d" % i, [1, 1024], BF16, ese) for i in range(2)]
                    wr_b = k.sb("wr_b", [128, 8, NEXP], BF16, ese)
                    br_b = k.sb("br_b", [1, NEXP], BF16, ese)
                    bgu = k.sb("bgu", [128, NEXP, 16], F32, ese)
                    modf = [k.sb("modf%d" % i, [128, D], F32, ese) for i in range(3)]
                    k.dma("pool", lambda e: e.dma_start(out=wr_b[:], in_=wr_d.rearrange("(kc p) e -> p kc e", p=128)), writes=[wr_b])
                    k.dma("pool", lambda e: e.dma_start(out=br_b[:], in_=br_d), writes=[br_b])
                    k.dma("sp", lambda e: e.dma_start(out=bgu[:], in_=bgu_d), writes=[bgu])
                    with ExitStack() as esi:
                        condbc = k.sb("condbc", [128, 8, 128], F32, esi)
                        wst = k.sb("wst", [128, 8, 256], F32, esi)
                        nrm_bc = k.sb("nrm_bc", [128, D], F32, esi)
                        k.op("dve", lambda e: e.tensor_copy(out=condbc[:], in_=cs[:].unsqueeze(2).broadcast_to([128, 8, 128])),
                             reads=[cs], writes=[condbc])
                        compute_mod(b, 3, modf, condbc, wst)
                        k.dma("sp", lambda e: e.dma_start(out=nrm_bc[:], in_=bc(nffn_d)), writes=[nrm_bc])
                        k.op("dve", lambda e: e.scalar_tensor_tensor(out=modf[1][:], in0=modf[1][:], scalar=1.0, in1=nrm_bc[:],
                                                                     op0=ALU.add, op1=ALU.mult), reads=[modf[1], nrm_bc], writes=[modf[1]])
                        k.barrier()
                    gw = k.sb("gw", [128, NTB, NEXP], F32, ese)
                    lg = k.sb("lg", [128, NEXP], F32, ese)
                    top8 = k.sb("top8", [128, 8], F32, ese)
                    negm = k.sb("negm", [128, 1], F32, ese)
                    msk = k.sb("msk", [128, NEXP], F32, ese)
                    rsum = k.sb("rsum", [128, 1], F32, ese)
                    gt_ = [k.sb("gt%d" % i, [128, TGW], F32, ese) for i in range(2)]
                    ut_ = [k.sb("ut%d" % i, [128, TGW], F32, ese) for i in range(2)]
                    sgt = [k.sb("sgt%d" % i, [128, TGW], F32, ese) for i in range(2)]
                    mtmp = [k.sb("mtmp%d" % i, [128, 512], F32, ese) for i in range(2)]
                    mstg = [k.sb("mstg%d" % i, [128, 8, 256], F32, ese) for i in range(2)]
                    st["ms"] = 0
                    nfin_bc = k.sb("nfin_bc", [128, D], F32, ese)
                    k.dma("sp", lambda e: e.dma_start(out=nfin_bc[:], in_=bc(nfin_d)), writes=[nfin_bc])
                    print("sbuf remaining (moe)", nc.sbuf_bytes_remaining, flush=True)

                    for tb in range(NTB):
                        norm_mod_T(tb, modf[1], modf[0], h2T, tb * 128)
                        P = pf()

                        def fn(e):
                            for kc in range(8):
                                e.matmul(P[:, 0:NEXP], lhsT=h2T[:, kc, tb * 128:(tb + 1) * 128], rhs=wr_b[:, kc, :], start=(kc == 0), stop=False)
                            return e.matmul(P[:, 0:NEXP], lhsT=ones_b[0:1, :], rhs=br_b[0:1, :], start=False, stop=True)
                        k.op("pe", fn, reads=[h2T, wr_b, ones_b, br_b], writes=[P])
                        k.op("act", lambda e: e.copy(out=lg[:], in_=P[:, 0:NEXP]), reads=[P], writes=[lg])
                        k.op("dve", lambda e: e.max(out=top8[:], in_=lg[:]), reads=[lg], writes=[top8])
                        k.op("dve", lambda e: e.tensor_scalar(out=negm[:], in0=top8[:, 0:1], scalar1=-1.0, scalar2=None, op0=ALU.mult),
                             reads=[top8], writes=[negm])
                        k.op("dve", lambda e: e.tensor_scalar(out=msk[:], in0=lg[:], scalar1=top8[:, 3:4], scalar2=None, op0=ALU.is_ge),
                             reads=[lg, top8], writes=[msk])
                        k.op("act", lambda e: e.activation(out=lg[:], in_=lg[:], func=AF.Exp, bias=negm[:]), reads=[lg, negm], writes=[lg])
                        k.op("dve", lambda e: e.tensor_tensor(out=lg[:], in0=lg[:], in1=msk[:], op=ALU.mult), reads=[lg, msk], writes=[lg])
                        k.op("dve", lambda e: e.tensor_reduce(out=rsum[:], in_=lg[:], axis=AX.X, op=ALU.add), reads=[lg], writes=[rsum])
                        k.op("dve", lambda e: e.reciprocal(out=rsum[:], in_=rsum[:]), reads=[rsum], writes=[rsum])
                        k.op("dve", lambda e: e.tensor_scalar(out=gw[:, tb, :], in0=lg[:], scalar1=rsum[:, 0:1], scalar2=None, op0=ALU.mult),
                             reads=[lg, rsum], writes=[gw])
                    if dbg and b == 0 and u == 0:
                        k.dma("sp", lambda e: e.dma_start(out=dbg_d["d_gw"], in_=gw[:]), reads=[gw])

                    for ex in range(NEXP):
                        Wg = wgu[0]
                        Wd = wdn[0]
                        Bd = bdn[ex % 2]
                        for q8 in range(8):
                            sg_ = mstg[st["ms"] % 2]
                            st["ms"] += 1
                            k.dma("sp", lambda e: e.dma_start(out=sg_[:], in_=wgu_d[ex, :, q8 * 256:(q8 + 1) * 256].rearrange(
                                "(kc p) c -> p kc c", p=128)), writes=[sg_])
                            k.op("act", lambda e: e.copy(out=Wg[:, :, q8 * 256:(q8 + 1) * 256], in_=sg_[:]), reads=[sg_], writes=[Wg])
                        for q8 in range(4):
                            sg_ = mstg[st["ms"] % 2]
                            st["ms"] += 1
                            k.dma("sp", lambda e: e.dma_start(out=sg_[:], in_=wd_d[ex, :, q8 * 256:(q8 + 1) * 256].rearrange(
                                "(kc p) c -> p kc c", p=128)), writes=[sg_])
                            k.op("dve", lambda e: e.tensor_tensor(out=Wd[:, :, q8 * 256:(q8 + 1) * 256], in0=sg_[:],
                                                                  in1=modf[2][:, q8 * 256:(q8 + 1) * 256].unsqueeze(1).broadcast_to([128, 8, 256]),
                                                                  op=ALU.mult), reads=[sg_, modf[2]], writes=[Wd])
                        k.dma("pool", lambda e: e.dma_start(out=Bd[:], in_=bd_d[ex:ex + 1, :]), writes=[Bd])
                        k.op("dve", lambda e: e.tensor_tensor(out=Bd[:], in0=Bd[:], in1=modf[2][0:1, :], op=ALU.mult), reads=[Bd, modf[2]], writes=[Bd])
                        for fc in range(8):
                            for tg in range(NTG):
                                tgs = slice(tg * TGW, (tg + 1) * TGW)
                                Pg = pf()
                                Pu = pf()

                                def fn(e):
                                    for kc in range(8):
                                        e.matmul(Pg[:, 0:TGW], lhsT=Wg[:, kc, fc * 128:(fc + 1) * 128], rhs=h2T[:, kc, tgs], start=(kc == 0), stop=(kc == 7))
                                    for kc in range(8):
                                        ins = e.matmul(Pu[:, 0:TGW], lhsT=Wg[:, kc, 1024 + fc * 128:1024 + (fc + 1) * 128], rhs=h2T[:, kc, tgs],
                                                       start=(kc == 0), stop=(kc == 7))
                                    return ins
                                k.op("pe", fn, reads=[Wg, h2T], writes=[Pg, Pu])
                                i2 = (fc * NTG + tg) % 2
                                G, Uu, Sg = gt_[i2], ut_[i2], sgt[i2]
                                k.op("dve", lambda e: e.tensor_scalar(out=G[:], in0=Pg[:, 0:TGW], scalar1=bgu[:, ex, fc:fc + 1], scalar2=7.0,
                                                                      op0=ALU.add, op1=ALU.min), reads=[Pg, bgu], writes=[G])
                                k.op("act", lambda e: e.activation(out=Uu[:], in_=Pu[:, 0:TGW], func=AF.Identity, bias=bgu[:, ex, 8 + fc:9 + fc]),
                                     reads=[Pu, bgu], writes=[Uu])
                                k.op("act", lambda e: e.activation(out=Sg[:], in_=G[:], func=AF.Sigmoid, scale=1.702), reads=[G], writes=[Sg])
                                k.op("dve", lambda e: e.tensor_scalar(out=Uu[:], in0=Uu[:], scalar1=-7.0, scalar2=7.0, op0=ALU.max, op1=ALU.min),
                                     reads=[Uu], writes=[Uu])
                                k.op("dve", lambda e: e.tensor_tensor(out=G[:], in0=G[:], in1=Sg[:], op=ALU.mult), reads=[G, Sg], writes=[G])
                                k.op("dve", lambda e: e.scalar_tensor_tensor(out=actT[:, fc, tgs], in0=Uu[:], scalar=1.0, in1=G[:],
                                                                             op0=ALU.add, op1=ALU.mult), reads=[Uu, G], writes=[actT])
                        for tb in range(NTB):
                            for nb in range(2):
                                P = pf()

                                def fn(e):
                                    for fc in range(8):
                                        e.matmul(P[:], lhsT=actT[:, fc, tb * 128:(tb + 1) * 128], rhs=Wd[:, fc, nb * 512:(nb + 1) * 512],
                                                 start=(fc == 0), stop=False)
                                    return e.matmul(P[:], lhsT=ones_b[0:1, :], rhs=Bd[0:1, nb * 512:(nb + 1) * 512], start=False, stop=True)
                                k.op("pe", fn, reads=[actT, Wd, ones_b, Bd], writes=[P])
                                k.op("dve", lambda e: e.scalar_tensor_tensor(out=acc[:, tb, nb * 512:(nb + 1) * 512], in0=P[:],
                                                                             scalar=gw[:, tb, ex:ex + 1],
                                                                             in1=acc[:, tb, nb * 512:(nb + 1) * 512], op0=ALU.mult, op1=ALU.add),
                                     reads=[P, gw, acc], writes=[acc])
                    for tb in range(NTB):
                        rms_rstd(acc[:, tb, :], acc, D, 3)
                        k.op("dve", lambda e: e.scalar_tensor_tensor(out=acc[:, tb, :], in0=acc[:, tb, :], scalar=rstd[:, 3:4], in1=nfin_bc[:],
                                                                     op0=ALU.mult, op1=ALU.mult), reads=[acc, rstd, nfin_bc], writes=[acc])
                    k.dma("sp", lambda e: e.dma_start(out=out_d[b, u * U:(u + 1) * U, :].rearrange("(tb p) d -> p tb d", p=128), in_=acc[:]),
                          reads=[acc])
                    k.barrier()
        k.finish()
        print("instr counts", k.ninstr, flush=True)
    return nc


def prep_inputs(inp, NSEQ, S, ncores):
    consts, gC = host_consts(S)
    f = lambda a: np.ascontiguousarray(a, dtype=np.float32)
    shared = {
        "w_ada": f(inp["w_ada"][0]), "b_ada": f(inp["b_ada"][0][None, :]),
        "norm_mix": f(inp["norm_mix"][0][None, :]), "norm_ffn": f(inp["norm_ffn"][0][None, :]),
        "norm_final": f(inp["norm_final"][None, :]), "ssm_norm": f(inp["ssm_norm"][0][None, :]),
        "dt_bias": f(inp["dt_bias"][0][None, :]), "a_log": f(inp["a_log"][0][None, :]), "d_skip": f(inp["d_skip"][0][None, :]),
        "w_in": f(inp["w_in"][0]),
        "cw": f(inp["conv_w"][0].reshape(4, 32, 128).transpose(2, 1, 0)),
        "cb": f(inp["conv_b"][0].reshape(32, 128).T),
        "w_ret_out": f(inp["w_ret_out"][0]), "w_ssm_out": f(inp["w_ssm_out"][0]), "w_out": f(inp["w_out"][0]),
        "w_router": f(inp["w_router"][0]), "b_router": f(inp["b_router"][0][None, :]),
        "w_gate_up": f(inp["w_gate_up"][0]),
        "bgu": f(inp["b_gate_up"][0].reshape(NEXP, 16, 128).transpose(2, 0, 1)),
        "w_down": f(inp["w_down"][0]), "b_down": f(inp["b_down"][0]),
    }
    shared.update(consts)
    x = np.asarray(inp["x"], dtype=np.float32)
    c = np.asarray(inp["c"], dtype=np.float32)
    maps = []
    for i in range(ncores):
        m = dict(shared)
        m["x"] = np.ascontiguousarray(x[i * NSEQ:(i + 1) * NSEQ])
        m["cl"] = f(c[i * NSEQ:(i + 1) * NSEQ].reshape(NSEQ, 8, 128).transpose(0, 2, 1))
        maps.append(m)
    return maps, gC


def kernel(**inputs):
    B, S, _ = inputs["x"].shape
    ncores = 8
    NSEQ = B // ncores
    maps, gC = prep_inputs(inputs, NSEQ, S, ncores)
    nc = build(NSEQ, S, gC)
    res = run_bass_kernel_spmd(nc, maps, core_ids=list(range(ncores)))
    out = np.concatenate([np.asarray(r["out"]) for r in res.results], axis=0)
    return out.astype(np.float32)
```
